# Optimizing a Trainium2 kernel written in Bass

```python
import math
import jax, jax.numpy as jnp
from jax import lax
import numpy as np

D_MODEL = 1024
BATCH = 8
SEQ = 4096
DEPTH = 2

N_META = 16
D_MIX = D_MODEL
CONV_DIM = D_MIX // 4
ATT_HEAD_DIM = 64
ATT_DIM = D_MIX // 2
ATT_HEADS = ATT_DIM // ATT_HEAD_DIM
SSM_DIM = D_MIX - CONV_DIM - ATT_DIM
SSM_GROUP = 16
SSM_GROUPS = SSM_DIM // SSM_GROUP
SSM_STATE = 64
CONV_WIDTH = 31
BLOCK_Q = 128
N_EXPERT_GROUPS = 4
EXPERTS_PER_GROUP = 4
N_EXPERTS = N_EXPERT_GROUPS * EXPERTS_PER_GROUP
TOP_K_INNER = 2
D_FF_EXPERT = D_MODEL // 4
EPS = 1e-6
NEG_BIG = -1e30

IN_WIDTHS = (CONV_DIM, CONV_DIM, ATT_DIM, ATT_DIM, ATT_DIM, ATT_HEADS, SSM_DIM)
IN_SPLITS = tuple(int(v) for v in np.cumsum(IN_WIDTHS)[:-1])
D_IN = int(sum(IN_WIDTHS))

kernel_name = "hymba_conformer_fox_s5_hiermoe"


def rms_norm(x, g):
    xf = x.astype(jnp.float32)
    y = xf * lax.rsqrt(jnp.mean(xf * xf, axis=-1, keepdims=True) + EPS)
    return (y * g.astype(jnp.float32)).astype(x.dtype)


def conformer_conv(a, gate, w_dw, b_dw, ln_g, ln_b):
    u = a * jax.nn.sigmoid(gate)
    u_pad = jnp.pad(u, ((0, 0), (CONV_WIDTH - 1, 0), (0, 0)))
    y = lax.conv_general_dilated(
        u_pad, w_dw[:, None, :].astype(u.dtype), window_strides=(1,), padding="VALID",
        dimension_numbers=("NWC", "WIO", "NWC"), feature_group_count=CONV_DIM)
    y = y.astype(jnp.float32) + b_dw.astype(jnp.float32)
    mu = jnp.mean(y, axis=-1, keepdims=True)
    var = jnp.mean(jnp.square(y - mu), axis=-1, keepdims=True)
    y = (y - mu) * lax.rsqrt(var + EPS) * ln_g.astype(jnp.float32) + ln_b.astype(jnp.float32)
    return jax.nn.silu(y).astype(a.dtype)


def forgetting_attention(q, k, v, f_logit):
    bsz, t_len = q.shape[0], q.shape[1]
    pad = BLOCK_Q - N_META
    l_len = t_len + pad
    n_blocks = l_len // BLOCK_Q
    cum = jnp.cumsum(jax.nn.log_sigmoid(f_logit.astype(jnp.float32)), axis=1)
    padt = lambda z: jnp.pad(z, ((0, 0), (pad, 0)) + ((0, 0),) * (z.ndim - 2))
    q, k, v, cum = padt(q), padt(k), padt(v), padt(cum)
    cum_k = cum.transpose(0, 2, 1)
    qb = q.reshape(bsz, n_blocks, BLOCK_Q, ATT_HEADS, ATT_HEAD_DIM).transpose(1, 0, 2, 3, 4)
    cqb = cum_k.reshape(bsz, ATT_HEADS, n_blocks, BLOCK_Q).transpose(2, 0, 1, 3)
    kpos = jnp.arange(l_len)
    scale = ATT_HEAD_DIM ** -0.5

    def one_block(args):
        i, qi, ci = args
        s = jnp.einsum("bqhd,bkhd->bhqk", qi, k, preferred_element_type=jnp.float32) * scale
        s = s + ci[..., None] - cum_k[:, :, None, :]
        qpos = i * BLOCK_Q + jnp.arange(BLOCK_Q)
        mask = (kpos[None, :] <= qpos[:, None]) & (kpos[None, :] >= pad)
        s = jnp.where(mask[None, None], s, NEG_BIG)
        p = jax.nn.softmax(s, axis=-1)
        return jnp.einsum("bhqk,bkhd->bqhd", p.astype(v.dtype), v)

    out = lax.map(one_block, (jnp.arange(n_blocks), qb, cqb))
    out = out.transpose(1, 0, 2, 3, 4).reshape(bsz, l_len, ATT_DIM)
    return out[:, pad:]


def _cplx_combine(e1, e2):
    a1r, a1i, b1r, b1i = e1
    a2r, a2i, b2r, b2i = e2
    ar = a2r * a1r - a2i * a1i
    ai = a2r * a1i + a2i * a1r
    br = a2r * b1r - a2i * b1i + b2r
    bi = a2r * b1i + a2i * b1r + b2i
    return ar, ai, br, bi


def s5_mixer(u, lam_re, lam_im, log_dt, b_re, b_im, c_re, c_im, d_skip, glu_w, glu_b):
    bsz, t_len = u.shape[0], u.shape[1]
    ug = u.reshape(bsz, t_len, SSM_GROUPS, SSM_GROUP).astype(jnp.float32)
    dt = jnp.exp(log_dt.astype(jnp.float32))[:, None]
    lr, li = lam_re.astype(jnp.float32), lam_im.astype(jnp.float32)
    mag = jnp.exp(lr * dt)
    ab_re, ab_im = mag * jnp.cos(li * dt), mag * jnp.sin(li * dt)
    nr, ni = ab_re - 1.0, ab_im
    den = lr * lr + li * li
    f_re, f_im = (nr * lr + ni * li) / den, (ni * lr - nr * li) / den
    br, bi = b_re.astype(jnp.float32), b_im.astype(jnp.float32)
    bb_re = f_re[..., None] * br - f_im[..., None] * bi
    bb_im = f_re[..., None] * bi + f_im[..., None] * br
    bu_re = jnp.einsum("btgh,gph->tbgp", ug, bb_re)
    bu_im = jnp.einsum("btgh,gph->tbgp", ug, bb_im)
    a_re = jnp.broadcast_to(ab_re[None, None], (t_len, 1, SSM_GROUPS, SSM_STATE))
    a_im = jnp.broadcast_to(ab_im[None, None], (t_len, 1, SSM_GROUPS, SSM_STATE))
    _, _, x_re, x_im = lax.associative_scan(_cplx_combine, (a_re, a_im, bu_re, bu_im), axis=0)
    y = (jnp.einsum("gho,tbgo->btgh", c_re.astype(jnp.float32), x_re)
         - jnp.einsum("gho,tbgo->btgh", c_im.astype(jnp.float32), x_im))
    y = y + d_skip.astype(jnp.float32).reshape(SSM_GROUPS, SSM_GROUP) * ug
    zg = jax.nn.gelu(y)
    gate = jnp.einsum("btgh,ghk->btgk", zg, glu_w.astype(jnp.float32)) + glu_b.astype(jnp.float32)
    out = zg * jax.nn.sigmoid(gate)
    return out.reshape(bsz, t_len, SSM_DIM).astype(u.dtype)


def hier_moe(h, wg, bg, we, be, w1, w3, w2):
    bsz, t_len, d = h.shape
    t = h.reshape(-1, d)
    lg = (t @ wg).astype(jnp.float32) + bg.astype(jnp.float32)
    pg = jax.nn.softmax(lg, axis=-1)
    g_sel = jnp.argmax(lg, axis=-1)
    pg_sel = jnp.take_along_axis(pg, g_sel[:, None], axis=-1)
    le = ((t @ we).astype(jnp.float32) + be.astype(jnp.float32)).reshape(-1, N_EXPERT_GROUPS, EXPERTS_PER_GROUP)
    le_sel = jnp.take_along_axis(le, g_sel[:, None, None], axis=1)[:, 0]
    top_v, top_i = lax.top_k(le_sel, TOP_K_INNER)
    pe = jax.nn.softmax(top_v, axis=-1) * pg_sel
    e_idx = g_sel[:, None] * EXPERTS_PER_GROUP + top_i
    gates = jnp.sum(jax.nn.one_hot(e_idx, N_EXPERTS, dtype=jnp.float32) * pe[..., None], axis=1)
    out = jnp.zeros(t.shape, jnp.float32)
    for e in range(N_EXPERTS):
        y = (jax.nn.silu(t @ w1[e]) * (t @ w3[e])) @ w2[e]
        out = out + gates[:, e:e + 1] * y.astype(jnp.float32)
    return out.reshape(bsz, t_len, d).astype(h.dtype)


def setup_inputs(seed: int = 0) -> dict:
    key = jax.random.key(seed)
    ks = jax.random.split(key, 32)
    nrm = lambda k, shape, s: jax.random.normal(k, shape, jnp.float32) * s
    L, D = DEPTH, D_MODEL
    n_idx = jnp.arange(SSM_STATE, dtype=jnp.float32)
    lam_im = jnp.broadcast_to(math.pi * n_idx, (L, SSM_GROUPS, SSM_STATE)) + nrm(ks[9], (L, SSM_GROUPS, SSM_STATE), 0.01)
    lam_re = -0.5 + nrm(ks[10], (L, SSM_GROUPS, SSM_STATE), 0.01)
    log_dt = jax.random.uniform(ks[11], (L, SSM_GROUPS), jnp.float32, math.log(1e-3), math.log(1e-1))
    return {
        "x": nrm(ks[0], (BATCH, SEQ, D), 1.0),
        "meta_tokens": nrm(ks[1], (N_META, D), 1.0),
        "norm_mix_g": 1.0 + nrm(ks[2], (L, D), 0.02),
        "w_in": nrm(ks[3], (L, D, D_IN), D ** -0.5),
        "fgate_b": 3.0 + nrm(ks[4], (L, ATT_HEADS), 1.0),
        "conv_w": nrm(ks[5], (L, CONV_WIDTH, CONV_DIM), CONV_WIDTH ** -0.5),
        "conv_b": nrm(ks[6], (L, CONV_DIM), 0.02),
        "conv_ln_g": 1.0 + nrm(ks[7], (L, CONV_DIM), 0.02),
        "conv_ln_b": nrm(ks[8], (L, CONV_DIM), 0.02),
        "att_norm_g": 1.0 + nrm(ks[12], (L, ATT_DIM), 0.02),
        "ssm_lam_re": lam_re,
        "ssm_lam_im": lam_im,
        "ssm_log_dt": log_dt,
        "ssm_b_re": nrm(ks[13], (L, SSM_GROUPS, SSM_STATE, SSM_GROUP), (2 * SSM_GROUP) ** -0.5),
        "ssm_b_im": nrm(ks[14], (L, SSM_GROUPS, SSM_STATE, SSM_GROUP), (2 * SSM_GROUP) ** -0.5),
        "ssm_c_re": nrm(ks[15], (L, SSM_GROUPS, SSM_GROUP, SSM_STATE), (2 * SSM_STATE) ** -0.5),
        "ssm_c_im": nrm(ks[16], (L, SSM_GROUPS, SSM_GROUP, SSM_STATE), (2 * SSM_STATE) ** -0.5),
        "ssm_d": nrm(ks[17], (L, SSM_DIM), 1.0),
        "ssm_glu_w": nrm(ks[18], (L, SSM_GROUPS, SSM_GROUP, SSM_GROUP), SSM_GROUP ** -0.5),
        "ssm_glu_b": nrm(ks[19], (L, SSM_GROUPS, SSM_GROUP), 0.02),
        "ssm_norm_g": 1.0 + nrm(ks[20], (L, SSM_DIM), 0.02),
        "w_out": nrm(ks[21], (L, D_MIX, D), D_MIX ** -0.5),
        "norm_ffn_g": 1.0 + nrm(ks[22], (L, D), 0.02),
        "router_g_w": nrm(ks[23], (L, D, N_EXPERT_GROUPS), D ** -0.5),
        "router_g_b": nrm(ks[24], (L, N_EXPERT_GROUPS), 0.01),
        "router_e_w": nrm(ks[25], (L, D, N_EXPERTS), D ** -0.5),
        "router_e_b": nrm(ks[26], (L, N_EXPERTS), 0.01),
        "exp_w1": nrm(ks[27], (L, N_EXPERTS, D, D_FF_EXPERT), D ** -0.5),
        "exp_w3": nrm(ks[28], (L, N_EXPERTS, D, D_FF_EXPERT), D ** -0.5),
        "exp_w2": nrm(ks[29], (L, N_EXPERTS, D_FF_EXPERT, D), D_FF_EXPERT ** -0.5),
        "final_norm_g": 1.0 + nrm(ks[30], (D,), 0.02),
    }


def reference(x, meta_tokens, norm_mix_g, w_in, fgate_b, conv_w, conv_b, conv_ln_g, conv_ln_b,
              att_norm_g, ssm_lam_re, ssm_lam_im, ssm_log_dt, ssm_b_re, ssm_b_im, ssm_c_re,
              ssm_c_im, ssm_d, ssm_glu_w, ssm_glu_b, ssm_norm_g, w_out, norm_ffn_g,
              router_g_w, router_g_b, router_e_w, router_e_b, exp_w1, exp_w3, exp_w2,
              final_norm_g):
    bsz = x.shape[0]
    meta = jnp.broadcast_to(meta_tokens[None].astype(x.dtype), (bsz, N_META, D_MODEL))
    h = jnp.concatenate([meta, x], axis=1)
    t_len = h.shape[1]
    for l in range(DEPTH):
        z = rms_norm(h, norm_mix_g[l])
        proj = z @ w_in[l]
        cv_a, cv_g, q, k, v, f_lg, s_u = jnp.split(proj, IN_SPLITS, axis=-1)
        y_conv = conformer_conv(cv_a, cv_g, conv_w[l], conv_b[l], conv_ln_g[l], conv_ln_b[l])
        hs = (bsz, t_len, ATT_HEADS, ATT_HEAD_DIM)
        y_att = forgetting_attention(q.reshape(hs), k.reshape(hs), v.reshape(hs),
                                     f_lg + fgate_b[l].astype(f_lg.dtype))
        y_ssm = s5_mixer(s_u, ssm_lam_re[l], ssm_lam_im[l], ssm_log_dt[l], ssm_b_re[l], ssm_b_im[l],
                         ssm_c_re[l], ssm_c_im[l], ssm_d[l], ssm_glu_w[l], ssm_glu_b[l])
        mixed = jnp.concatenate([y_conv, rms_norm(y_att, att_norm_g[l]),
                                 rms_norm(y_ssm, ssm_norm_g[l])], axis=-1)
        h = h + mixed @ w_out[l]
        h = h + hier_moe(rms_norm(h, norm_ffn_g[l]), router_g_w[l], router_g_b[l],
                         router_e_w[l], router_e_b[l], exp_w1[l], exp_w3[l], exp_w2[l])
    return rms_norm(h, final_norm_g)[:, N_META:]
```

```python
import contextlib
import math
import numpy as np
import concourse.bass as bass
import concourse.mybir as mybir
from concourse.bass_utils import run_bass_kernel_spmd

F32 = mybir.dt.float32
BF16 = mybir.dt.bfloat16
I32 = mybir.dt.int32
AF = mybir.ActivationFunctionType
ALU = mybir.AluOpType
AX = mybir.AxisListType

D = 1024
DIN = 2312
NH = 8
EPS = 1e-6
CW = 31
NE = 16
PI = math.pi


class Res:
    __slots__ = ("name", "last_w", "readers")

    def __init__(self, name=""):
        self.name = name
        self.last_w = None
        self.readers = []


class Op:
    __slots__ = ("eng", "fn", "deps", "sig", "sigval", "dma", "dsem", "dval", "idx")


class Prog:
    ENGS = ("pe", "act", "dve", "pool", "sp")
    QS = ("sp", "pool", "act")

    def __init__(self, nc, stack, ring=16):
        self.nc = nc
        self.ops = []
        self.ring = ring
        self.handles = {"pe": nc.tensor, "act": nc.scalar, "dve": nc.vector,
                        "pool": nc.gpsimd, "sp": nc.sync}
        self.esem = {e: stack.enter_context(nc.semaphore("s_" + e)) for e in self.ENGS}
        self.rings = {q: [stack.enter_context(nc.semaphore("r_%s%d" % (q, i))) for i in range(ring)]
                      for q in self.QS}
        self.cnt = {e: 0 for e in self.ENGS}
        self.dcnt = {q: 0 for q in self.QS}
        self.seen = {e: {} for e in self.ENGS}
        self.touched = set()
        self.nop_total = 0
        self.nwait = 0

    def op(self, eng, fn, reads=(), writes=(), dma=False):
        o = Op()
        o.eng = eng
        o.fn = fn
        o.dma = dma
        o.sig = dma
        o.sigval = 0
        o.idx = len(self.ops)
        deps = {}
        for r in reads:
            if r.last_w is not None:
                deps[id(r.last_w)] = r.last_w
        for w in writes:
            if w.last_w is not None:
                deps[id(w.last_w)] = w.last_w
            for rd in w.readers:
                deps[id(rd)] = rd
        dl = []
        for d in deps.values():
            if d is o:
                continue
            if eng == "pe" and d.eng == "pe" and not d.dma:
                continue
            d.sig = True
            dl.append(d)
        o.deps = dl
        for r in reads:
            r.readers.append(o)
            self.touched.add(r)
        for w in writes:
            w.last_w = o
            w.readers = []
            self.touched.add(w)
        self.ops.append(o)
        return o

    def pe(self, fn, reads=(), writes=()):
        return self.op("pe", fn, reads, writes)

    def act(self, fn, reads=(), writes=()):
        return self.op("act", fn, reads, writes)

    def dve(self, fn, reads=(), writes=()):
        return self.op("dve", fn, reads, writes)

    def pool(self, fn, reads=(), writes=()):
        return self.op("pool", fn, reads, writes)

    def dma(self, q, out, in_, reads=(), writes=(), **kw):
        return self.op(q, lambda e: e.dma_start(out=out, in_=in_, allow_slow_non_contiguous=True, **kw),
                       reads, writes, dma=True)

    def side_begin(self):
        self._saved = self.ops
        self.ops = []

    def side_end(self):
        side = self.ops
        self.ops = self._saved
        return side

    def flush(self):
        ring = self.ring
        last = {}
        for o in self.ops:
            if not o.dma:
                last[o.eng] = o
        for o in last.values():
            o.sig = True
        for o in self.ops:
            if o.dma:
                j = self.dcnt[o.eng]
                self.dcnt[o.eng] += 1
                o.dsem = (o.eng, j % ring)
                o.dval = 16 * (j // ring + 1)
            elif o.sig:
                self.cnt[o.eng] += 1
                o.sigval = self.cnt[o.eng]
        for o in self.ops:
            h = self.handles[o.eng]
            sn = self.seen[o.eng]
            if o.dma:
                q, slot = o.dsem
                prev = o.dval - 16
                if prev > 0 and sn.get(("r", q, slot), 0) < prev:
                    h.wait_ge(self.rings[q][slot], prev)
                    sn[("r", q, slot)] = prev
                    self.nwait += 1
            for d in o.deps:
                if d.dma:
                    key = ("r",) + d.dsem
                    val = d.dval
                    sem = self.rings[d.dsem[0]][d.dsem[1]]
                else:
                    key = ("e", d.eng)
                    val = d.sigval
                    sem = self.esem[d.eng]
                if sn.get(key, 0) >= val:
                    continue
                h.wait_ge(sem, val)
                sn[key] = val
                self.nwait += 1
            ins = o.fn(h)
            if o.dma:
                ins.then_inc(self.rings[o.dsem[0]][o.dsem[1]], 16)
            elif o.sig:
                ins.then_inc(self.esem[o.eng], 1)
        self.nop_total += len(self.ops)
        self.ops = []
        sp = self.handles["sp"]
        sn = self.seen["sp"]
        for e in self.ENGS:
            if e == "sp":
                continue
            v = self.cnt[e]
            if v > 0 and sn.get(("e", e), 0) < v:
                sp.wait_ge(self.esem[e], v)
                sn[("e", e)] = v
        for q in self.QS:
            n = self.dcnt[q]
            for slot in range(ring):
                uses = (n - slot + ring - 1) // ring if n > slot else 0
                v = 16 * uses
                if v > 0 and sn.get(("r", q, slot), 0) < v:
                    sp.wait_ge(self.rings[q][slot], v)
                    sn[("r", q, slot)] = v
        sp.sem_inc(self.esem["sp"], 1)
        self.cnt["sp"] += 1
        bv = self.cnt["sp"]
        for e in self.ENGS:
            s2 = self.seen[e]
            if e != "sp":
                self.handles[e].wait_ge(self.esem["sp"], bv)
            for e2 in self.ENGS:
                s2[("e", e2)] = self.cnt[e2]
            for q in self.QS:
                n = self.dcnt[q]
                for slot in range(ring):
                    uses = (n - slot + ring - 1) // ring if n > slot else 0
                    s2[("r", q, slot)] = 16 * uses
        for r in self.touched:
            r.last_w = None
            r.readers = []
        self.touched = set()


PARAM_SHAPES = {
    "meta_tokens": (16, 1024), "norm_mix_g": (2, 1024), "w_in": (2, 1024, 2312), "fgate_b": (2, 8),
    "conv_w": (2, 31, 256), "conv_b": (2, 256), "conv_ln_g": (2, 256), "conv_ln_b": (2, 256),
    "att_norm_g": (2, 512), "ssm_lam_re": (2, 16, 64), "ssm_lam_im": (2, 16, 64), "ssm_log_dt": (2, 16),
    "ssm_b_re": (2, 16, 64, 16), "ssm_b_im": (2, 16, 64, 16), "ssm_c_re": (2, 16, 16, 64),
    "ssm_c_im": (2, 16, 16, 64), "ssm_d": (2, 256), "ssm_glu_w": (2, 16, 16, 16), "ssm_glu_b": (2, 16, 16),
    "ssm_norm_g": (2, 256), "w_out": (2, 1024, 1024), "norm_ffn_g": (2, 1024), "router_g_w": (2, 1024, 4),
    "router_g_b": (2, 4), "router_e_w": (2, 1024, 16), "router_e_b": (2, 16),
    "exp_w1": (2, 16, 1024, 256), "exp_w3": (2, 16, 1024, 256), "exp_w2": (2, 16, 256, 1024),
    "final_norm_g": (1024,),
}


def host_consts():
    ident = np.eye(128, dtype=np.float32)
    p = np.arange(128)[:, None]
    f = np.arange(128)[None, :]
    maskneg = np.where(p <= f, 0.0, -30000.0).astype(np.float32)
    iota = np.broadcast_to(np.arange(128, dtype=np.float32)[None, :], (128, 128)).copy()
    onesm = np.ones((128, 128), np.float32)
    onesm[:, 0] = 0.0
    return {"c_ident": ident, "c_maskneg": maskneg, "c_iota": iota, "c_onesm": onesm}


def build(SEQ, L=2, debug=False, stop_after=None):
    T = SEQ + 16
    NT = (T + 127) // 128
    TP = NT * 128
    blocks = [(t0, min(512, TP - t0)) for t0 in range(0, TP, 512)]
    nc = bass.Bass("TRN2", target_bir_lowering=False)
    dbgkind = "ExternalOutput" if debug else "Internal"

    def din(name, shape, dt=F32):
        return nc.dram_tensor(name, list(shape), dt, kind="ExternalInput").ap()

    def dscr(name, shape, dt, dbg=False):
        if dbg and debug:
            return nc.dram_tensor(name, list(shape), dt, kind="ExternalOutput").ap()
        return nc.dram_tensor(name, list(shape), dt).ap()

    x = din("x", (SEQ, D))
    prm = {k: din(k, v) for k, v in PARAM_SHAPES.items()}
    cst = {k: din(k, (128, 128)) for k in ("c_ident", "c_maskneg", "c_iota", "c_onesm")}
    out = nc.dram_tensor("out", [SEQ, D], F32, kind="ExternalOutput").ap()

    h_d = dscr("h_d", (TP, D), F32, dbg=True)
    qT_d = dscr("qT_d", (NH, 70, TP), BF16, dbg=True)
    kT_d = dscr("kT_d", (NH, 70, TP), BF16, dbg=True)
    v_d = dscr("v_d", (NH, TP, 64), BF16, dbg=True)
    mixT_d = dscr("mixT_d", (D, TP), BF16, dbg=True)
    rs_d = dscr("rs_d", (128, NT * 9), F32, dbg=True)
    suT_d = dscr("suT_d", (256, TP), BF16)
    win_b = dscr("win_b", (L, D, DIN), BF16)
    wout_b = dscr("wout_b", (L, D, D), BF16)
    w13_b = dscr("w13_b", (L, NE, D, 512), BF16)
    w2_b = dscr("w2_b", (L, NE, 256, D), BF16)

    with contextlib.ExitStack() as gst:
        P = Prog(nc, gst)
        R_h = [Res("h%d" % i) for i in range(NT)]
        R_q, R_k, R_v, R_mix = Res("qT"), Res("kT"), Res("vd"), Res("mix")
        R_sud = Res("sud")
        R_mixc, R_mixa, R_mixs = Res("mixc"), Res("mixa"), Res("mixs")
        R_win = [Res("win%d" % l) for l in range(L)]
        R_wout = [Res("wout%d" % l) for l in range(L)]
        R_w13 = [Res("w13_%d" % l) for l in range(L)]
        R_w2 = [Res("w2_%d" % l) for l in range(L)]

        uid = {"n": 0}

        def mk(st, name, shape, dt):
            uid["n"] += 1
            return st.enter_context(nc.sbuf_tensor("%s_%d" % (name, uid["n"]), list(shape), dt)), Res(name)

        psb = [gst.enter_context(nc.psum_tensor("psb%d" % i, [128, 512], F32)) for i in range(8)]
        R_ps = [Res("ps%d" % i) for i in range(8)]
        R_ps7 = [R_ps[7]] * 4
        ident, R_ident = mk(gst, "ident", (128, 128), F32)
        rsatt, R_rsatt = mk(gst, "rsatt", (128, NT, NH), F32)
        rsssm, R_rsssm = mk(gst, "rsssm", (128, NT), F32)
        onescol, R_onescol = mk(gst, "onescol", (128, 1), F32)

        def cast_layer_small(l):
            for kc in range(8):
                P.dma("pool", win_b[l, kc * 128:(kc + 1) * 128, :], prm["w_in"][l, kc * 128:(kc + 1) * 128, :],
                      writes=[R_win[l]])

        def cast_layer_big(l):
            for kc in range(4):
                P.dma("pool", wout_b[l, kc * 256:(kc + 1) * 256, :], prm["w_out"][l, kc * 256:(kc + 1) * 256, :],
                      writes=[R_wout[l]])
            for e in range(NE):
                P.dma("pool", w13_b[l, e, :, 0:256], prm["exp_w1"][l, e], writes=[R_w13[l]])
                P.dma("pool", w13_b[l, e, :, 256:512], prm["exp_w3"][l, e], writes=[R_w13[l]])
                P.dma("pool", w2_b[l, e], prm["exp_w2"][l, e], writes=[R_w2[l]])

        with contextlib.ExitStack() as st:
            zt, R_zt = mk(st, "zt", (128, D), F32)
            onesb, R_onesb = mk(st, "onesb", (NH, 3, TP), BF16)
            P.dma("sp", ident[:], cst["c_ident"], writes=[R_ident])
            P.dve(lambda e: e.memset(zt[:], 0.0), writes=[R_zt])
            P.dve(lambda e: e.memset(onesb[:], 1.0), writes=[R_onesb])
            P.dve(lambda e: e.memset(onescol[:], 1.0), writes=[R_onescol])
            if TP > T:
                P.dma("sp", h_d[T:TP, :], zt[0:TP - T, :], reads=[R_zt], writes=R_h)
            P.dma("sp", qT_d[:, 67:70, :], onesb[:], reads=[R_onesb], writes=[R_q])
            P.dma("sp", kT_d[:, 64:67, :], onesb[:], reads=[R_onesb], writes=[R_k])
            P.flush()

        def load_h(l, dst, R_dst, ti):
            if l > 0:
                P.dma("sp", dst[:], h_d[ti * 128:(ti + 1) * 128, :], reads=[R_h[ti]], writes=[R_dst])
                return
            lo, hi = ti * 128, (ti + 1) * 128
            if ti == 0:
                P.dma("sp", dst[0:16, :], prm["meta_tokens"], writes=[R_dst])
                P.dma("sp", dst[16:128, :], x[0:112, :], writes=[R_dst])
            elif hi <= T:
                P.dma("sp", dst[:], x[lo - 16:hi - 16, :], writes=[R_dst])
            else:
                nv = T - lo
                P.dma("sp", dst[0:nv, :], x[lo - 16:T - 16, :], writes=[R_dst])
                P.dma("sp", dst[nv:128, :], h_d[T:TP, :], reads=[R_h[ti]], writes=[R_dst])

        for l in range(L):
            with contextlib.ExitStack() as st:
                stq_a = "pool" if l > 0 else "sp"
                win, R_wint = mk(st, "win", (128, 8, DIN), BF16)
                gb, R_gb = mk(st, "gb", (128, D), F32)
                hbuf = [mk(st, "hbuf%d" % i, (128, D), F32) for i in range(2)]
                junk, R_junk = mk(st, "junk", (128, D), BF16)
                znb = [mk(st, "znb%d" % i, (128, D), F32) for i in range(2)]
                zT = [mk(st, "zT%d" % i, (128, 8, 512), BF16) for i in range(2)]
                st8 = [mk(st, "st8_%d" % i, (128, 8), F32) for i in range(2)]
                uT, R_uT = mk(st, "uT", (128, 2, TP + 32), BF16)
                sg, R_sg = mk(st, "sg", (128, 512), F32)
                qkst = [mk(st, "qkst%d" % i, (128, 512), BF16) for i in range(2)]
                vst = [mk(st, "vst%d" % i, (128, 512), BF16) for i in range(2)]
                negb, R_negb = mk(st, "negb", (NH, 1), F32)
                fe, R_fe = mk(st, "fe", (NH, 512), F32)
                ones8, R_ones8 = mk(st, "ones8", (NH, 512), F32)
                crow, R_crow = mk(st, "crow", (NH, TP), F32)
                cres, R_cres = mk(st, "cres", (NH, 512), F32)
                cpos, R_cpos = mk(st, "cpos", (NH, 3, 512), BF16)
                cneg, R_cneg = mk(st, "cneg", (NH, 3, 512), BF16)
                cwl, R_cwl = mk(st, "cwl", (CW, 256), F32)
                cwT, R_cwT = mk(st, "cwT", (128, 2, 32), F32)
                diagw, R_diagw = mk(st, "diagw", (128, 2, CW, 128), BF16)
                cvp, R_cvp = mk(st, "cvp", (128, 2, 4), F32)
                ycv, R_ycv = mk(st, "ycv", (128, 2, 512), F32)
                ysq, R_ysq = mk(st, "ysq", (128, 2, 512), F32)
                onesf, R_onesf = mk(st, "onesf", (128, 128), F32)
                mean, R_mean = mk(st, "mean", (128, 512), F32)
                var, R_var = mk(st, "var", (128, 512), F32)
                rstd, R_rstd = mk(st, "rstd", (128, 512), F32)
                cvo = [mk(st, "cvo%d" % i, (128, 512), BF16) for i in range(2)]
                sust = [mk(st, "sust%d" % i, (128, 512), BF16) for i in range(2)]

                if l == 0:
                    wstg = [mk(st, "wstg%d" % i, (128, DIN), F32) for i in range(2)]
                for kc in range(8):
                    if l == 0:
                        ws_, R_ws = wstg[kc % 2]
                        P.dma("sp", ws_[:], prm["w_in"][l, kc * 128:(kc + 1) * 128, :], writes=[R_ws])
                        if kc % 2 == 0:
                            P.dve(lambda e, ws_=ws_, kc=kc: e.tensor_copy(out=win[:, kc, :], in_=ws_[:]), reads=[R_ws], writes=[R_wint])
                        else:
                            P.act(lambda e, ws_=ws_, kc=kc: e.copy(out=win[:, kc, :], in_=ws_[:]), reads=[R_ws], writes=[R_wint])
                    else:
                        P.dma("sp", win[:, kc, :], win_b[l, kc * 128:(kc + 1) * 128, :], reads=[R_win[l]], writes=[R_wint])
                P.dma("sp", gb[:], prm["norm_mix_g"][l:l + 1, :].partition_broadcast(128), writes=[R_gb])
                P.dma("sp", negb[:], prm["fgate_b"][l:l + 1, :].rearrange("o h -> h o"), writes=[R_negb])
                P.act(lambda e: e.mul(out=negb[:], in_=negb[:], mul=-1.0), reads=[R_negb], writes=[R_negb])
                P.dve(lambda e: e.memset(ones8[:], 1.0), writes=[R_ones8])
                P.dve(lambda e: e.memset(onesf[:], 1.0 / 256.0), writes=[R_onesf])
                P.dve(lambda e: e.memset(uT[:, :, 0:32], 0.0), writes=[R_uT])
                P.dma("sp", cwl[:], prm["conv_w"][l], writes=[R_cwl])
                for hf in range(2):
                    P.pe(lambda e, hf=hf: e.transpose(psb[7][:, 0:CW], cwl[:, hf * 128:(hf + 1) * 128], ident[0:CW, 0:CW]),
                         reads=[R_cwl, R_ident], writes=[R_ps[7]])
                    P.dve(lambda e, hf=hf: e.tensor_copy(out=cwT[:, hf, 0:CW], in_=psb[7][:, 0:CW]),
                          reads=[R_ps[7]], writes=[R_cwT])
                for hf in range(2):
                    for j in range(CW):
                        P.dve(lambda e, hf=hf, j=j: e.tensor_scalar(out=diagw[:, hf, j, :], in0=ident[:],
                                                                    scalar1=cwT[:, hf, j:j + 1], scalar2=None, op0=ALU.mult),
                              reads=[R_ident, R_cwT], writes=[R_diagw])
                for i, nm in enumerate(("conv_b", "conv_ln_g", "conv_ln_b")):
                    P.dma("sp", cvp[:, :, i], prm[nm][l].rearrange("(hf p) -> p hf", p=128), writes=[R_cvp])
                pass

                if l == 0:
                    cast_layer_big(0)
                cstate = {"n": 0}

                def prep(b):
                    t0, W = blocks[b]
                    zt_, R_z = zT[b % 2]
                    for s in range(W // 128):
                        i = cstate["n"]
                        cstate["n"] += 1
                        ht, R_ht = hbuf[i % 2]
                        zn, R_zn = znb[i % 2]
                        s8, R_s8 = st8[i % 2]
                        ti = (t0 + s * 128) // 128
                        load_h(l, ht, R_ht, ti)
                        P.act(lambda e, ht=ht, s8=s8: e.activation(out=junk[:], in_=ht[:], func=AF.Square, accum_out=s8[:, 0:1]),
                              reads=[R_ht], writes=[R_junk, R_s8])
                        P.act(lambda e, s8=s8: e.activation(out=s8[:, 1:2], in_=s8[:, 0:1], func=AF.Sqrt, scale=1.0 / D, bias=EPS),
                              reads=[R_s8], writes=[R_s8])
                        P.dve(lambda e, s8=s8: e.reciprocal(out=s8[:, 2:3], in_=s8[:, 1:2]), reads=[R_s8], writes=[R_s8])
                        P.dve(lambda e, ht=ht, zn=zn, s8=s8: e.scalar_tensor_tensor(out=zn[:], in0=ht[:], scalar=s8[:, 2:3], in1=gb[:],
                                                                                   op0=ALU.mult, op1=ALU.mult),
                              reads=[R_ht, R_s8, R_gb], writes=[R_zn])
                        for half in range(2):
                            for c4 in range(4):
                                kc = half * 4 + c4
                                P.pe(lambda e, zn=zn, kc=kc, c4=c4, half=half: e.transpose(
                                    psb[half][:, c4 * 128:(c4 + 1) * 128], zn[:, kc * 128:(kc + 1) * 128], ident[:]),
                                    reads=[R_zn, R_ident], writes=[R_ps[half]])
                        P.act(lambda e, zt_=zt_, s=s: e.copy(out=zt_[:, 0:4, s * 128:(s + 1) * 128],
                                                             in_=psb[0][:].rearrange("p (c t) -> p c t", c=4)),
                              reads=[R_ps[0]], writes=[R_z])
                        P.dve(lambda e, zt_=zt_, s=s: e.tensor_copy(out=zt_[:, 4:8, s * 128:(s + 1) * 128],
                                                                    in_=psb[1][:].rearrange("p (c t) -> p c t", c=4)),
                              reads=[R_ps[1]], writes=[R_z])

                def proj_mm(bank, c0, ncol, zt_, R_z, W):
                    for kc in range(8):
                        P.pe(lambda e, kc=kc: e.matmul(psb[bank][0:ncol, 0:W], lhsT=win[:, kc, c0:c0 + ncol], rhs=zt_[:, kc, 0:W],
                                                       start=(kc == 0), stop=(kc == 7)),
                             reads=[R_wint, R_z], writes=[R_ps[bank]])

                rot = {"n": 0, "q": 0, "v": 0, "c": 0}
                lnq = {"ops": []}

                def lnpull(n=1):
                    if lnq["ops"]:
                        P.ops.extend(lnq["ops"][:n])
                        lnq["ops"] = lnq["ops"][n:]

                def main(b):
                    t0, W = blocks[b]
                    zt_, R_z = zT[b % 2]
                    for hf in range(2):
                        proj_mm(2, hf * 128, 128, zt_, R_z, W)
                        proj_mm(3, 256 + hf * 128, 128, zt_, R_z, W)
                        P.act(lambda e: e.activation(out=sg[:, 0:W], in_=psb[3][:, 0:W], func=AF.Sigmoid),
                              reads=[R_ps[3]], writes=[R_sg])
                        P.dve(lambda e, hf=hf: e.tensor_tensor(out=uT[:, hf, 32 + t0:32 + t0 + W], in0=psb[2][:, 0:W], in1=sg[:, 0:W], op=ALU.mult),
                              reads=[R_ps[2], R_sg], writes=[R_uT])
                    for kind in range(2):
                        for c in range(4):
                            bank = 4 + rot["n"] % 2
                            rot["n"] += 1
                            lnpull()
                            proj_mm(bank, 512 + kind * 512 + c * 128, 128, zt_, R_z, W)
                            stg, R_stg = qkst[rot["q"] % 2]
                            rot["q"] += 1
                            if kind == 0:
                                P.act(lambda e, stg=stg, bank=bank: e.mul(out=stg[:, 0:W], in_=psb[bank][:, 0:W], mul=0.125),
                                      reads=[R_ps[bank]], writes=[R_stg])
                            else:
                                P.dve(lambda e, stg=stg, bank=bank: e.tensor_copy(out=stg[:, 0:W], in_=psb[bank][:, 0:W]),
                                      reads=[R_ps[bank]], writes=[R_stg])
                            dst, R_dst = (qT_d, R_q) if kind == 0 else (kT_d, R_k)
                            for hh in range(2):
                                P.dma(stq_a, dst[2 * c + hh, 0:64, t0:t0 + W], stg[hh * 64:(hh + 1) * 64, 0:W],
                                      reads=[R_stg], writes=[R_dst])
                    for s in range(W // 128):
                        bank = 4 + rot["n"] % 2
                        rot["n"] += 1
                        lnpull()
                        for kc in range(8):
                            P.pe(lambda e, kc=kc, s=s, bank=bank: e.matmul(psb[bank][:, :], lhsT=zt_[:, kc, s * 128:(s + 1) * 128],
                                                                rhs=win[:, kc, 1536:2048], start=(kc == 0), stop=(kc == 7)),
                                 reads=[R_wint, R_z], writes=[R_ps[bank]])
                        vt, R_vt = vst[rot["v"] % 2]
                        rot["v"] += 1
                        if s % 2 == 0:
                            P.act(lambda e, vt=vt, bank=bank: e.copy(out=vt[:], in_=psb[bank][:]), reads=[R_ps[bank]], writes=[R_vt])
                        else:
                            P.dve(lambda e, vt=vt, bank=bank: e.tensor_copy(out=vt[:], in_=psb[bank][:]), reads=[R_ps[bank]], writes=[R_vt])
                        tt = t0 + s * 128
                        P.dma(stq_a, v_d[:, tt:tt + 128, :].rearrange("h t d -> t h d"),
                              vt[:].rearrange("t (h d) -> t h d", h=NH), reads=[R_vt], writes=[R_v])
                    fbank = 4 + rot["n"] % 2
                    rot["n"] += 1
                    lnpull()
                    proj_mm(fbank, 2048, NH, zt_, R_z, W)
                    P.act(lambda e: e.activation(out=fe[:, 0:W], in_=psb[fbank][0:NH, 0:W], func=AF.Exp, scale=-1.0, bias=negb[:, 0:1]),
                          reads=[R_ps[fbank], R_negb], writes=[R_fe])
                    P.act(lambda e: e.activation(out=fe[:, 0:W], in_=fe[:, 0:W], func=AF.Ln, bias=1.0),
                          reads=[R_fe], writes=[R_fe])
                    init = 0.0 if t0 == 0 else crow[:, t0 - 1:t0]
                    P.dve(lambda e, init=init: e.tensor_tensor_scan(out=crow[:, t0:t0 + W], data0=ones8[:, 0:W], data1=fe[:, 0:W],
                                                                    initial=init, op0=ALU.mult, op1=ALU.subtract),
                          reads=[R_ones8, R_fe, R_crow], writes=[R_crow])
                    P.dve(lambda e: e.tensor_copy(out=cpos[:, 0, 0:W], in_=crow[:, t0:t0 + W]), reads=[R_crow], writes=[R_cpos])
                    P.dve(lambda e: e.tensor_tensor(out=cres[:, 0:W], in0=crow[:, t0:t0 + W], in1=cpos[:, 0, 0:W], op=ALU.subtract),
                          reads=[R_crow, R_cpos], writes=[R_cres])
                    P.dve(lambda e: e.tensor_copy(out=cpos[:, 1, 0:W], in_=cres[:, 0:W]), reads=[R_cres], writes=[R_cpos])
                    P.dve(lambda e: e.tensor_tensor(out=cres[:, 0:W], in0=cres[:, 0:W], in1=cpos[:, 1, 0:W], op=ALU.subtract),
                          reads=[R_cres, R_cpos], writes=[R_cres])
                    P.dve(lambda e: e.tensor_copy(out=cpos[:, 2, 0:W], in_=cres[:, 0:W]), reads=[R_cres], writes=[R_cpos])
                    P.act(lambda e: e.mul(out=cneg[:, :, 0:W], in_=cpos[:, :, 0:W], mul=-1.0), reads=[R_cpos], writes=[R_cneg])
                    P.dma(stq_a, qT_d[:, 64:67, t0:t0 + W], cpos[:, :, 0:W], reads=[R_cpos], writes=[R_q])
                    P.dma(stq_a, kT_d[:, 67:70, t0:t0 + W], cneg[:, :, 0:W], reads=[R_cneg], writes=[R_k])
                    for hf in range(2):
                        bank = 4 + rot["n"] % 2
                        rot["n"] += 1
                        proj_mm(bank, 2056 + hf * 128, 128, zt_, R_z, W)
                        su_, R_su_ = sust[hf]
                        P.act(lambda e, su_=su_, bank=bank: e.copy(out=su_[:, 0:W], in_=psb[bank][:, 0:W]),
                              reads=[R_ps[bank]], writes=[R_su_])
                        P.dma(stq_a, suT_d[hf * 128:(hf + 1) * 128, t0:t0 + W], su_[:, 0:W], reads=[R_su_], writes=[R_sud])
                    lnpull(1000)
                    for hf in range(2):
                        bank = 2 + hf
                        for j in range(CW):
                            P.pe(lambda e, hf=hf, j=j, bank=bank: e.matmul(psb[bank][:, 0:W], lhsT=diagw[:, hf, j, :],
                                                                           rhs=uT[:, hf, t0 + j + 2:t0 + j + 2 + W],
                                                                           start=(j == 0), stop=(j == CW - 1)),
                                 reads=[R_diagw, R_uT], writes=[R_ps[bank]])
                        P.act(lambda e, hf=hf, bank=bank: e.activation(out=ycv[:, hf, 0:W], in_=psb[bank][:, 0:W], func=AF.Identity,
                                                                       bias=cvp[:, hf, 0:1]),
                              reads=[R_ps[bank], R_cvp], writes=[R_ycv])
                        P.act(lambda e, hf=hf, bank=bank: e.activation(out=ysq[:, hf, 0:W], in_=psb[bank][:, 0:W], func=AF.Square,
                                                                       bias=cvp[:, hf, 0:1]),
                              reads=[R_ps[bank], R_cvp], writes=[R_ysq])
                    for hf in range(2):
                        P.pe(lambda e, hf=hf: e.matmul(psb[6][:, 0:W], lhsT=onesf[:], rhs=ycv[:, hf, 0:W], start=(hf == 0), stop=(hf == 1)),
                             reads=[R_onesf, R_ycv], writes=[R_ps[6]])
                    for hf in range(2):
                        P.pe(lambda e, hf=hf: e.matmul(psb[7][:, 0:W], lhsT=onesf[:], rhs=ysq[:, hf, 0:W], start=(hf == 0), stop=(hf == 1)),
                             reads=[R_onesf, R_ysq], writes=[R_ps[7]])
                    P.side_begin()
                    P.act(lambda e: e.copy(out=mean[:, 0:W], in_=psb[6][:, 0:W]), reads=[R_ps[6]], writes=[R_mean])
                    P.dve(lambda e: e.tensor_tensor(out=var[:, 0:W], in0=mean[:, 0:W], in1=mean[:, 0:W], op=ALU.mult),
                          reads=[R_mean], writes=[R_var])
                    P.dve(lambda e: e.tensor_tensor(out=var[:, 0:W], in0=psb[7][:, 0:W], in1=var[:, 0:W], op=ALU.subtract),
                          reads=[R_ps[7], R_var], writes=[R_var])
                    P.act(lambda e: e.activation(out=var[:, 0:W], in_=var[:, 0:W], func=AF.Sqrt, bias=EPS),
                          reads=[R_var], writes=[R_var])
                    P.dve(lambda e: e.reciprocal(out=rstd[:, 0:W], in_=var[:, 0:W]), reads=[R_var], writes=[R_rstd])
                    for hf in range(2):
                        P.dve(lambda e, hf=hf: e.tensor_tensor(out=ycv[:, hf, 0:W], in0=ycv[:, hf, 0:W], in1=mean[:, 0:W], op=ALU.subtract),
                              reads=[R_ycv, R_mean], writes=[R_ycv])
                        P.dve(lambda e, hf=hf: e.tensor_tensor(out=ycv[:, hf, 0:W], in0=ycv[:, hf, 0:W], in1=rstd[:, 0:W], op=ALU.mult),
                              reads=[R_ycv, R_rstd], writes=[R_ycv])
                        co, R_co = cvo[rot["c"] % 2]
                        rot["c"] += 1
                        P.act(lambda e, hf=hf, co=co: e.activation(out=co[:, 0:W], in_=ycv[:, hf, 0:W], func=AF.Silu,
                                                                   scale=cvp[:, hf, 1:2], bias=cvp[:, hf, 2:3]),
                              reads=[R_ycv, R_cvp], writes=[R_co])
                        P.dma(stq_a, mixT_d[hf * 128:(hf + 1) * 128, t0:t0 + W], co[:, 0:W], reads=[R_co], writes=[R_mixc])
                    lnq["ops"] = P.side_end()

                prep(0)
                for b in range(len(blocks)):
                    if b + 1 < len(blocks):
                        prep(b + 1)
                    main(b)
                P.ops.extend(lnq["ops"])
                lnq["ops"] = []
                P.flush()
            if stop_after == ("A", l):
                break

            with contextlib.ExitStack() as st:
                if l + 1 < L:
                    cast_layer_small(l + 1)
                    cast_layer_big(l + 1)
                maskneg, R_mask = mk(st, "maskneg", (128, 128), F32)
                iota, R_iota = mk(st, "iota", (128, 128), F32)
                onesm, R_onesm = mk(st, "onesm", (128, 128), F32)
                for nm, tl, rr in (("c_maskneg", maskneg, R_mask), ("c_iota", iota, R_iota), ("c_onesm", onesm, R_onesm)):
                    P.dma("sp", tl[:], cst[nm], writes=[rr])
                Kt = [mk(st, "Kt%d" % i, (70, TP), BF16) for i in range(2)]
                Qt = [mk(st, "Qt%d" % i, (70, TP), BF16) for i in range(2)]
                Vt = [mk(st, "Vt%d" % i, (128, NT, 65), BF16) for i in range(2)]
                PT = [mk(st, "PT%d" % i, (128, 512), BF16) for i in range(3)]
                identb, R_identb = mk(st, "identb", (128, 128), BF16)
                maskb, R_maskb = mk(st, "maskb", (128, 128), BF16)
                P.dve(lambda e: e.tensor_copy(out=identb[:], in_=ident[:]), reads=[R_ident], writes=[R_identb])
                P.dve(lambda e: e.tensor_copy(out=maskb[:], in_=maskneg[:]), reads=[R_mask], writes=[R_maskb])
                ot, R_ot = mk(st, "ot", (65, 512), F32)
                rl, R_rl = mk(st, "rl", (64, 512), F32)
                yt, R_yt = mk(st, "yt", (64, 512), F32)
                ysq2, R_ysq2 = mk(st, "ysq2", (64, 512), F32)
                ybf = [mk(st, "ybf%d" % i, (64, 512), BF16) for i in range(2)]
                sel, R_sel = mk(st, "sel", (65, 64), F32)
                tl4, R_tl4 = mk(st, "tl4", (128, 4), F32)
                gatt, R_gatt = mk(st, "gatt", (64, NH), F32)
                P.dve(lambda e: e.memset(sel[:], 0.0), writes=[R_sel])
                P.dve(lambda e: e.memset(sel[64:65, :], 1.0), writes=[R_sel])
                for i in range(2):
                    P.dve(lambda e, i=i: e.memset(Vt[i][0][:, :, 64:65], 1.0), writes=[Vt[i][1]])
                P.dma("sp", gatt[:], prm["att_norm_g"][l].rearrange("(h d) -> d h", d=64), writes=[R_gatt])
                cnt = {"s": 0, "p": 0, "m": 0, "y": 0}
                inter = {"side": [], "pos": 0, "per": 0, "pair": 0, "lastdue": 0, "blk": 0}
                epi_q = []

                def epi_tick():
                    inter["pair"] += 1
                    while epi_q and epi_q[0][1] <= inter["pair"]:
                        P.ops.append(epi_q.pop(0)[0])

                def pull_side():
                    sd = inter["side"]
                    p0 = inter["pos"]
                    if p0 < inter["nset"]:
                        p1 = p0 + 1
                    else:
                        inter["acc"] += inter["rate"]
                        n = int(inter["acc"])
                        inter["acc"] -= n
                        p1 = min(len(sd), p0 + n)
                    if p1 > p0:
                        P.ops.extend(sd[p0:p1])
                        inter["pos"] = p1
                def att_block(hd, K_, R_K, Q_, R_Q, V_, R_V, t0, W):
                    while epi_q and epi_q[0][2] <= inter["blk"] - 2:
                        P.ops.append(epi_q.pop(0)[0])
                    nk = (t0 + W) // 128
                    obank = 2 + (cnt["y"] % 2)

                    def s_mm(ki):
                        k0 = ki * 128
                        qs = max(t0, k0)
                        Wq = t0 + W - qs
                        bank = cnt["s"] % 2
                        cnt["s"] += 1
                        diag = (k0 >= t0)
                        P.pe(lambda e: e.matmul(psb[bank][:, 0:Wq], lhsT=K_[:, k0:k0 + 128], rhs=Q_[:, qs:qs + Wq], start=True, stop=(not diag)),
                             reads=[R_K, R_Q], writes=[R_ps[bank]])
                        if diag:
                            P.pe(lambda e: e.matmul(psb[bank][:, 0:128], lhsT=identb[:], rhs=maskb[:], start=False, stop=True),
                                 reads=[R_identb, R_maskb], writes=[R_ps[bank]])
                        pt, R_pt = PT[cnt["p"] % 3]
                        cnt["p"] += 1
                        P.act(lambda e: e.activation(out=pt[:, 0:Wq], in_=psb[bank][:, 0:Wq], func=AF.Exp),
                              reads=[R_ps[bank]], writes=[R_pt])
                        return (ki, qs, Wq, pt, R_pt)

                    def pv_mm(info):
                        ki, qs, Wq, pt, R_pt = info
                        P.pe(lambda e: e.matmul(psb[obank][0:65, qs - t0:qs - t0 + Wq], lhsT=V_[:, ki, :], rhs=pt[:, 0:Wq],
                                                start=(ki == 0), stop=(ki == nk - 1)),
                             reads=[R_V, R_pt], writes=[R_ps[obank]])

                    pend = s_mm(0)
                    for ki in range(nk):
                        nxt = s_mm(ki + 1) if ki + 1 < nk else None
                        pv_mm(pend)
                        pend = nxt
                        pull_side()
                        epi_tick()
                    cnt["y"] += 1
                    P.side_begin()
                    ns = W // 128
                    ti0 = t0 // 128
                    P.act(lambda e: e.copy(out=ot[:, 0:W], in_=psb[obank][0:65, 0:W]), reads=[R_ps[obank]], writes=[R_ot])
                    P.act(lambda e: e.activation(out=ysq2[:, 0:W], in_=ot[0:64, 0:W], func=AF.Square), reads=[R_ot], writes=[R_ysq2])
                    P.pe(lambda e: e.matmul(psb[obank][0:64, 0:W], lhsT=sel[:], rhs=ot[:, 0:W], start=True, stop=True),
                         reads=[R_sel, R_ot], writes=[R_ps[obank]])
                    for s in range(ns):
                        P.pe(lambda e, s=s: e.matmul(psb[4][:, s:s + 1], lhsT=ysq2[:, s * 128:(s + 1) * 128], rhs=onescol[0:64, :],
                                                     start=True, stop=True),
                             reads=[R_ysq2, R_onescol], writes=[R_ps[4]])
                    for s in range(ns):
                        P.pe(lambda e, s=s: e.matmul(psb[4][:, 4 + s:5 + s], lhsT=ot[:, s * 128:(s + 1) * 128], rhs=sel[:, 0:1],
                                                     start=True, stop=True),
                             reads=[R_ot, R_sel], writes=[R_ps[4]])
                    P.dve(lambda e: e.reciprocal(out=rl[:, 0:W], in_=psb[obank][0:64, 0:W]), reads=[R_ps[obank]], writes=[R_rl])
                    yb, R_yb = ybf[cnt["y"] % 2]
                    P.dve(lambda e: e.scalar_tensor_tensor(out=yb[:, 0:W], in0=ot[0:64, 0:W], scalar=gatt[:, hd:hd + 1], in1=rl[:, 0:W],
                                                           op0=ALU.mult, op1=ALU.mult),
                          reads=[R_ot, R_gatt, R_rl], writes=[R_yb])
                    P.dma("sp", mixT_d[256 + hd * 64:256 + (hd + 1) * 64, t0:t0 + W], yb[:, 0:W], reads=[R_yb], writes=[R_mixa])
                    P.dve(lambda e: e.reciprocal(out=tl4[:, 0:ns], in_=psb[4][:, 4:4 + ns]), reads=[R_ps[4]], writes=[R_tl4])
                    P.dve(lambda e: e.tensor_tensor(out=tl4[:, 0:ns], in0=tl4[:, 0:ns], in1=tl4[:, 0:ns], op=ALU.mult), reads=[R_tl4], writes=[R_tl4])
                    P.dve(lambda e: e.tensor_tensor(out=rsatt[:, ti0:ti0 + ns, hd], in0=psb[4][:, 0:ns], in1=tl4[:, 0:ns], op=ALU.mult),
                          reads=[R_ps[4], R_tl4], writes=[R_rsatt])
                    eops = P.side_end()
                    delays = [1, 1, 1] + [1] + [0] * (ns - 1) + [0] * ns + [1, 4, 1, 0, 0, 0]
                    assert len(delays) == len(eops), (len(delays), len(eops))
                    due = max(inter["pair"], inter["lastdue"])
                    for o_, d_ in zip(eops, delays):
                        due += d_
                        epi_q.append((o_, due, inter["blk"]))
                    inter["lastdue"] = due
                    inter["blk"] += 1

                def att_loads(hd):
                    K_, R_K = Kt[hd % 2]
                    Q_, R_Q = Qt[hd % 2]
                    V_, R_V = Vt[hd % 2]
                    P.dma("sp", K_[:], kT_d[hd], reads=[R_k], writes=[R_K])
                    P.dma("sp", Q_[:], qT_d[hd], reads=[R_q], writes=[R_Q])
                    P.dma("sp", V_[:, :, 0:64], v_d[hd].rearrange("(i p) d -> p i d", p=128), reads=[R_v], writes=[R_V])

                def run_attention(side):
                    npairs = NH * sum((t0 + W) // 128 for (t0, W) in blocks)
                    inter["side"] = side
                    inter["pos"] = 0
                    inter["nset"] = s5_nsetup
                    inter["rate"] = (len(side) - s5_nsetup) / max(1.0, 0.93 * npairs - s5_nsetup)
                    inter["acc"] = 0.0
                    att_loads(0)
                    for hd in range(NH):
                        if hd + 1 < NH:
                            att_loads(hd + 1)
                        K_, R_K = Kt[hd % 2]
                        Q_, R_Q = Qt[hd % 2]
                        V_, R_V = Vt[hd % 2]
                        for (t0, W) in blocks:
                            att_block(hd, K_, R_K, Q_, R_Q, V_, R_V, t0, W)
                    for q_ in epi_q:
                        P.ops.append(q_[0])
                    del epi_q[:]
                    P.ops.extend(side[inter["pos"]:])
                    inter["pos"] = len(side)

                P.side_begin()
                sp8 = {}
                for nm in ("lr", "li", "dt", "th", "r", "ang", "cs", "sn", "are", "aim", "nr", "den", "fre", "fim", "t1", "t2",
                           "xlr", "xli", "u1", "u2"):
                    sp8[nm] = mk(st, "s8_" + nm, (128, 8), F32)
                angi, R_angi = mk(st, "angi", (128, 8), I32)
                bre, R_bre = mk(st, "bre", (128, 8, 16), F32)
                bim, R_bim = mk(st, "bim", (128, 8, 16), F32)
                bbre, R_bbre = mk(st, "bbre", (128, 8, 16), F32)
                bbim, R_bbim = mk(st, "bbim", (128, 8, 16), F32)
                tb, R_tb = mk(st, "tb", (128, 8, 16), F32)
                Xb, R_Xb = mk(st, "Xb", (128, 16, 128), F32)
                Btab, R_Btab = mk(st, "Btab", (128, 16, 128), BF16)
                Yc = [mk(st, "Yc%d" % i, (128, 128), F32) for i in range(2)]
                cT = [mk(st, "cT%d" % i, (128, 8, 16), F32) for i in range(2)]
                Ctab, R_Ctab = mk(st, "Ctab", (128, 16, 128), BF16)
                dcol, R_dcol = mk(st, "dcol", (128, 2, 8), F32)
                Dtab, R_Dtab = mk(st, "Dtab", (128, 2, 128), BF16)
                Gf, R_Gf = mk(st, "Gf", (128, 2, 128), F32)
                Gtab, R_Gtab = mk(st, "Gtab", (128, 2, 128), BF16)
                ang, R_ang = mk(st, "angf", (128, 1024), F32)
                angn, R_angn = mk(st, "angn", (128, 1024), F32)
                angq, R_angq = mk(st, "angq", (128, 1024), I32)
                cosT, R_cos = mk(st, "cosT", (128, 1024), F32)
                sinT, R_sin = mk(st, "sinT", (128, 1024), F32)
                rtab, R_rtab = mk(st, "rtab", (128, 1024), F32)
                tA, R_tA = mk(st, "tA", (128, 1024), F32)
                tB, R_tB = mk(st, "tB", (128, 1024), F32)
                bpreL = [mk(st, "bpre%d" % i, (128, 1024), F32) for i in range(2)]
                bpimL = [mk(st, "bpim%d" % i, (128, 1024), F32) for i in range(2)]
                wreL = [mk(st, "wre%d" % i, (128, 1024), F32) for i in range(2)]
                wimL = [mk(st, "wim%d" % i, (128, 1024), F32) for i in range(2)]
                tC, R_tC = mk(st, "tC", (128, 1024), F32)
                tD, R_tD = mk(st, "tD", (128, 1024), F32)
                xreL = [mk(st, "xre%d" % i, (128, 1024), BF16) for i in range(2)]
                ximL = [mk(st, "xim%d" % i, (128, 1024), BF16) for i in range(2)]
                zgL = [mk(st, "zg%d" % i, (128, 2, 128), F32) for i in range(2)]
                zgbL = [mk(st, "zgb%d" % i, (128, 2, 128), BF16) for i in range(2)]
                sgt, R_sgt = mk(st, "sgt", (128, 2, 128), F32)
                th2, R_th2 = mk(st, "th2", (128, 2, 128), F32)
                yoL = [mk(st, "yo%d" % i, (128, 2, 128), F32) for i in range(2)]
                ysq3, R_ysq3 = mk(st, "ysq3", (128, 2, 128), F32)
                ybs = [mk(st, "ybs%d" % i, (128, 2, 128), BF16) for i in range(2)]
                suT, R_su = mk(st, "suT", (128, 2, TP), BF16)
                P.dma("sp", suT[:], suT_d.rearrange("(hf p) t -> p hf t", p=128), reads=[R_sud], writes=[R_su])

                def S(nm):
                    return sp8[nm][0]

                def RS(nm):
                    return sp8[nm][1]

                def tt8(o, a, b, op):
                    P.dve(lambda e: e.tensor_tensor(out=S(o)[:], in0=S(a)[:], in1=S(b)[:], op=op), reads=[RS(a), RS(b)], writes=[RS(o)])

                P.dma("sp", S("lr")[:], prm["ssm_lam_re"][l].rearrange("g p -> (g p)").rearrange("(k q) -> q k", q=128), writes=[RS("lr")])
                P.dma("sp", S("li")[:], prm["ssm_lam_im"][l].rearrange("g p -> (g p)").rearrange("(k q) -> q k", q=128), writes=[RS("li")])
                ldt = prm["ssm_log_dt"][l:l + 1, :].rearrange("o (k g2) -> o g2 k", g2=2)
                for g2 in range(2):
                    P.dma("sp", S("dt")[g2 * 64:(g2 + 1) * 64, :], ldt[:, g2, :].partition_broadcast(64), writes=[RS("dt")])
                P.dma("sp", bre[:], prm["ssm_b_re"][l].rearrange("g p h -> (g p) h").rearrange("(k q) h -> q k h", q=128), writes=[R_bre])
                P.dma("sp", bim[:], prm["ssm_b_im"][l].rearrange("g p h -> (g p) h").rearrange("(k q) h -> q k h", q=128), writes=[R_bim])
                for ri, nm in enumerate(("ssm_c_re", "ssm_c_im")):
                    src = prm[nm][l].rearrange("(k g2) ho p -> k ho g2 p", g2=2)
                    for k in range(8):
                        P.dma("sp", Yc[ri][0][k * 16:(k + 1) * 16, :].rearrange("ho (g2 p) -> ho g2 p", g2=2), src[k],
                              writes=[Yc[ri][1]])
                for i, nm in enumerate(("ssm_d", "ssm_norm_g")):
                    P.dma("sp", dcol[:, :, i], prm[nm][l].rearrange("(hf p) -> p hf", p=128), writes=[R_dcol])
                P.dma("sp", dcol[:, :, 2], prm["ssm_glu_b"][l].rearrange("g k -> (g k)").rearrange("(hf p) -> p hf", p=128), writes=[R_dcol])
                P.dve(lambda e: e.memset(Gf[:], 0.0), writes=[R_Gf])
                for g in range(16):
                    hf, gl = g // 8, g % 8
                    P.dma("sp", Gf[gl * 16:(gl + 1) * 16, hf, gl * 16:(gl + 1) * 16], prm["ssm_glu_w"][l, g], reads=[R_Gf], writes=[R_Gf])
                P.dve(lambda e: e.tensor_copy(out=Gtab[:], in_=Gf[:]), reads=[R_Gf], writes=[R_Gtab])
                P.dve(lambda e: e.tensor_scalar(out=dcol[:, :, 3], in0=dcol[:, :, 2], scalar1=0.5, scalar2=None, op0=ALU.mult),
                      reads=[R_dcol], writes=[R_dcol])
                P.dve(lambda e: e.tensor_scalar(out=dcol[:, :, 4], in0=dcol[:, :, 1], scalar1=0.25, scalar2=None, op0=ALU.mult),
                      reads=[R_dcol], writes=[R_dcol])
                for hf in range(2):
                    P.dve(lambda e, hf=hf: e.tensor_scalar(out=Dtab[:, hf, :], in0=ident[:], scalar1=dcol[:, hf, 0:1], scalar2=None, op0=ALU.mult),
                          reads=[R_ident, R_dcol], writes=[R_Dtab])
                P.act(lambda e: e.activation(out=S("dt")[:], in_=S("dt")[:], func=AF.Exp), reads=[RS("dt")], writes=[RS("dt")])
                tt8("t1", "lr", "dt", ALU.mult)
                P.act(lambda e: e.activation(out=S("r")[:], in_=S("t1")[:], func=AF.Exp), reads=[RS("t1")], writes=[RS("r")])
                tt8("th", "li", "dt", ALU.mult)

                def sincos(src, R_src, n, it, R_it, tmp, R_tmp, cs_out, R_cs, sn_out, R_sn):
                    P.dve(lambda e: e.tensor_scalar(out=tmp, in0=src, scalar1=1.0 / (2 * PI), scalar2=None, op0=ALU.mult),
                          reads=[R_src], writes=[R_tmp])
                    P.dve(lambda e: e.tensor_copy(out=it, in_=tmp), reads=[R_tmp], writes=[R_it])
                    P.dve(lambda e: e.tensor_copy(out=tmp, in_=it), reads=[R_it], writes=[R_tmp])
                    P.dve(lambda e: e.scalar_tensor_tensor(out=tmp, in0=tmp, scalar=-2 * PI, in1=src, op0=ALU.mult, op1=ALU.add),
                          reads=[R_tmp, R_src], writes=[R_tmp])
                    P.dve(lambda e: e.tensor_scalar(out=tmp, in0=tmp, scalar1=PI, scalar2=-PI, op0=ALU.min, op1=ALU.max),
                          reads=[R_tmp], writes=[R_tmp])
                    P.act(lambda e: e.activation(out=sn_out, in_=tmp, func=AF.Sin), reads=[R_tmp], writes=[R_sn])
                    P.act(lambda e: e.activation(out=tmp, in_=tmp, func=AF.Abs), reads=[R_tmp], writes=[R_tmp])
                    P.dve(lambda e: e.tensor_scalar(out=tmp, in0=tmp, scalar1=-1.0, scalar2=PI / 2, op0=ALU.mult, op1=ALU.add),
                          reads=[R_tmp], writes=[R_tmp])
                    P.act(lambda e: e.activation(out=cs_out, in_=tmp, func=AF.Sin), reads=[R_tmp], writes=[R_cs])

                sincos(S("th")[:], RS("th"), 8, angi[:], R_angi, S("ang")[:], RS("ang"), S("cs")[:], RS("cs"), S("sn")[:], RS("sn"))
                tt8("are", "r", "cs", ALU.mult)
                tt8("aim", "r", "sn", ALU.mult)
                P.dve(lambda e: e.tensor_scalar(out=S("nr")[:], in0=S("are")[:], scalar1=-1.0, scalar2=None, op0=ALU.add),
                      reads=[RS("are")], writes=[RS("nr")])
                tt8("t1", "lr", "lr", ALU.mult)
                tt8("t2", "li", "li", ALU.mult)
                tt8("den", "t1", "t2", ALU.add)
                P.dve(lambda e: e.reciprocal(out=S("den")[:], in_=S("den")[:]), reads=[RS("den")], writes=[RS("den")])
                tt8("t1", "nr", "lr", ALU.mult)
                tt8("t2", "aim", "li", ALU.mult)
                tt8("fre", "t1", "t2", ALU.add)
                tt8("fre", "fre", "den", ALU.mult)
                tt8("t1", "aim", "lr", ALU.mult)
                tt8("t2", "nr", "li", ALU.mult)
                tt8("fim", "t1", "t2", ALU.subtract)
                tt8("fim", "fim", "den", ALU.mult)
                for k in range(8):
                    P.dve(lambda e, k=k: e.tensor_scalar(out=tb[:, k, :], in0=bim[:, k, :], scalar1=S("fim")[:, k:k + 1], scalar2=None, op0=ALU.mult),
                          reads=[R_bim, RS("fim")], writes=[R_tb])
                    P.dve(lambda e, k=k: e.scalar_tensor_tensor(out=bbre[:, k, :], in0=bre[:, k, :], scalar=S("fre")[:, k:k + 1], in1=tb[:, k, :],
                                                                op0=ALU.mult, op1=ALU.subtract),
                          reads=[R_bre, RS("fre"), R_tb], writes=[R_bbre])
                    P.dve(lambda e, k=k: e.tensor_scalar(out=tb[:, k, :], in0=bre[:, k, :], scalar1=S("fim")[:, k:k + 1], scalar2=None, op0=ALU.mult),
                          reads=[R_bre, RS("fim")], writes=[R_tb])
                    P.dve(lambda e, k=k: e.scalar_tensor_tensor(out=bbim[:, k, :], in0=bim[:, k, :], scalar=S("fre")[:, k:k + 1], in1=tb[:, k, :],
                                                                op0=ALU.mult, op1=ALU.add),
                          reads=[R_bim, RS("fre"), R_tb], writes=[R_bbim])
                P.dve(lambda e: e.memset(Xb[:], 0.0), writes=[R_Xb])
                for k in range(8):
                    for ri, (src, R_src) in enumerate(((bbre, R_bbre), (bbim, R_bbim))):
                        c0 = 32 * (k % 4)
                        P.dve(lambda e, k=k, ri=ri, src=src, c0=c0: e.tensor_copy(out=Xb[0:64, 2 * k + ri, c0:c0 + 16], in_=src[0:64, k, :]),
                              reads=[R_src, R_Xb], writes=[R_Xb])
                        P.dve(lambda e, k=k, ri=ri, src=src, c0=c0: e.tensor_copy(out=Xb[64:128, 2 * k + ri, c0 + 16:c0 + 32], in_=src[64:128, k, :]),
                              reads=[R_src, R_Xb], writes=[R_Xb])
                for j in range(16):
                    bank = 5 + j % 2
                    P.pe(lambda e, j=j, bank=bank: e.transpose(psb[bank][:, 0:128], Xb[:, j, :], ident[:]),
                         reads=[R_Xb, R_ident], writes=[R_ps[bank]])
                    P.act(lambda e, j=j, bank=bank: e.copy(out=Btab[:, j, :], in_=psb[bank][:, 0:128]), reads=[R_ps[bank]], writes=[R_Btab])
                P.dve(lambda e: e.memset(Ctab[:], 0.0), writes=[R_Ctab])
                for ri in range(2):
                    P.pe(lambda e, ri=ri: e.transpose(psb[5 + ri][:, 0:128], Yc[ri][0][:], ident[:]),
                         reads=[Yc[ri][1], R_ident], writes=[R_ps[5 + ri]])
                    sc = 1.0 if ri == 0 else -1.0
                    P.act(lambda e, ri=ri, sc=sc: e.mul(out=cT[ri][0][:].rearrange("q k h -> q (k h)"), in_=psb[5 + ri][:, 0:128], mul=sc),
                          reads=[R_ps[5 + ri]], writes=[cT[ri][1]])
                    for k in range(8):
                        c0 = 32 * (k % 4)
                        P.dve(lambda e, k=k, ri=ri, c0=c0: e.tensor_copy(out=Ctab[0:64, 2 * k + ri, c0:c0 + 16], in_=cT[ri][0][0:64, k, :]),
                              reads=[cT[ri][1], R_Ctab], writes=[R_Ctab])
                        P.dve(lambda e, k=k, ri=ri, c0=c0: e.tensor_copy(out=Ctab[64:128, 2 * k + ri, c0 + 16:c0 + 32], in_=cT[ri][0][64:128, k, :]),
                              reads=[cT[ri][1], R_Ctab], writes=[R_Ctab])
                for k in range(8):
                    P.dve(lambda e, k=k: e.tensor_scalar(out=ang[:, k * 128:(k + 1) * 128], in0=iota[:], scalar1=S("th")[:, k:k + 1], scalar2=None,
                                                         op0=ALU.mult),
                          reads=[R_iota, RS("th")], writes=[R_ang])
                    P.dve(lambda e, k=k: e.tensor_scalar(out=rtab[:, k * 128:(k + 1) * 128], in0=onesm[:], scalar1=S("r")[:, k:k + 1], scalar2=None,
                                                         op0=ALU.mult),
                          reads=[R_onesm, RS("r")], writes=[R_rtab])
                sincos(ang[:], R_ang, 1024, angq[:], R_angq, angn[:], R_angn, cosT[:], R_cos, sinT[:], R_sin)

                def v3(t):
                    return t[:].rearrange("p (k i) -> p k i", k=8)

                def stA(c):
                    t0 = c * 128
                    bpre, R_bpre = bpreL[c % 2]
                    bpim, R_bpim = bpimL[c % 2]
                    for hb in range(2):
                        sl = slice(hb * 512, (hb + 1) * 512)
                        for kk in range(4):
                            k = hb * 4 + kk
                            for ri in range(2):
                                P.pe(lambda e, k=k, ri=ri, kk=kk, hb=hb: e.matmul(psb[5 + ri][:, kk * 128:(kk + 1) * 128], lhsT=Btab[:, 2 * k + ri, :],
                                                                                  rhs=suT[:, hb, t0:t0 + 128], start=True, stop=True),
                                     reads=[R_Btab, R_su], writes=[R_ps[5 + ri]])
                        P.dve(lambda e, sl=sl: e.tensor_tensor(out=tA[:, sl], in0=psb[5][:], in1=cosT[:, sl], op=ALU.mult),
                              reads=[R_ps[5], R_cos], writes=[R_tA])
                        P.dve(lambda e, sl=sl: e.tensor_tensor(out=tB[:, sl], in0=psb[6][:], in1=sinT[:, sl], op=ALU.mult),
                              reads=[R_ps[6], R_sin], writes=[R_tB])
                        P.dve(lambda e, sl=sl: e.tensor_tensor(out=bpre[:, sl], in0=tA[:, sl], in1=tB[:, sl], op=ALU.add),
                              reads=[R_tA, R_tB], writes=[R_bpre])
                        P.dve(lambda e, sl=sl: e.tensor_tensor(out=tA[:, sl], in0=psb[6][:], in1=cosT[:, sl], op=ALU.mult),
                              reads=[R_ps[6], R_cos], writes=[R_tA])
                        P.dve(lambda e, sl=sl: e.tensor_tensor(out=tB[:, sl], in0=psb[5][:], in1=sinT[:, sl], op=ALU.mult),
                              reads=[R_ps[5], R_sin], writes=[R_tB])
                        P.dve(lambda e, sl=sl: e.tensor_tensor(out=bpim[:, sl], in0=tA[:, sl], in1=tB[:, sl], op=ALU.subtract),
                              reads=[R_tA, R_tB], writes=[R_bpim])

                def stB(c):
                    bpre, R_bpre = bpreL[c % 2]
                    bpim, R_bpim = bpimL[c % 2]
                    wre, R_wre = wreL[c % 2]
                    wim, R_wim = wimL[c % 2]
                    if c > 0:
                        tt8("u1", "are", "xlr", ALU.mult)
                        tt8("u2", "aim", "xli", ALU.mult)
                        tt8("u1", "u1", "u2", ALU.subtract)
                        P.dve(lambda e: e.tensor_tensor(out=v3(bpre)[:, :, 0], in0=v3(bpre)[:, :, 0], in1=S("u1")[:], op=ALU.add),
                              reads=[R_bpre, RS("u1")], writes=[R_bpre])
                        tt8("u1", "are", "xli", ALU.mult)
                        tt8("u2", "aim", "xlr", ALU.mult)
                        tt8("u1", "u1", "u2", ALU.add)
                        P.dve(lambda e: e.tensor_tensor(out=v3(bpim)[:, :, 0], in0=v3(bpim)[:, :, 0], in1=S("u1")[:], op=ALU.add),
                              reads=[R_bpim, RS("u1")], writes=[R_bpim])
                    P.dve(lambda e: e.tensor_tensor_scan(out=wre[:], data0=rtab[:], data1=bpre[:], initial=0.0, op0=ALU.mult, op1=ALU.add),
                          reads=[R_rtab, R_bpre], writes=[R_wre])
                    P.dve(lambda e: e.tensor_tensor_scan(out=wim[:], data0=rtab[:], data1=bpim[:], initial=0.0, op0=ALU.mult, op1=ALU.add),
                          reads=[R_rtab, R_bpim], writes=[R_wim])
                    if c + 1 < NT:
                        P.dve(lambda e: e.tensor_tensor(out=S("t1")[:], in0=v3(wre)[:, :, 127], in1=v3(cosT)[:, :, 127], op=ALU.mult),
                              reads=[R_wre, R_cos], writes=[RS("t1")])
                        P.dve(lambda e: e.tensor_tensor(out=S("t2")[:], in0=v3(wim)[:, :, 127], in1=v3(sinT)[:, :, 127], op=ALU.mult),
                              reads=[R_wim, R_sin], writes=[RS("t2")])
                        tt8("xlr", "t1", "t2", ALU.subtract)
                        P.dve(lambda e: e.tensor_tensor(out=S("t1")[:], in0=v3(wre)[:, :, 127], in1=v3(sinT)[:, :, 127], op=ALU.mult),
                              reads=[R_wre, R_sin], writes=[RS("t1")])
                        P.dve(lambda e: e.tensor_tensor(out=S("t2")[:], in0=v3(wim)[:, :, 127], in1=v3(cosT)[:, :, 127], op=ALU.mult),
                              reads=[R_wim, R_cos], writes=[RS("t2")])
                        tt8("xli", "t1", "t2", ALU.add)

                def stC(c):
                    wre, R_wre = wreL[c % 2]
                    wim, R_wim = wimL[c % 2]
                    xre, R_xre = xreL[c % 2]
                    xim, R_xim = ximL[c % 2]
                    P.pool(lambda e: e.tensor_tensor(out=tC[:], in0=wre[:], in1=cosT[:], op=ALU.mult), reads=[R_wre, R_cos], writes=[R_tC])
                    P.pool(lambda e: e.tensor_tensor(out=tD[:], in0=wim[:], in1=sinT[:], op=ALU.mult), reads=[R_wim, R_sin], writes=[R_tD])
                    P.pool(lambda e: e.tensor_tensor(out=xre[:], in0=tC[:], in1=tD[:], op=ALU.subtract), reads=[R_tC, R_tD], writes=[R_xre])
                    P.pool(lambda e: e.tensor_tensor(out=tC[:], in0=wre[:], in1=sinT[:], op=ALU.mult), reads=[R_wre, R_sin], writes=[R_tC])
                    P.pool(lambda e: e.tensor_tensor(out=tD[:], in0=wim[:], in1=cosT[:], op=ALU.mult), reads=[R_wim, R_cos], writes=[R_tD])
                    P.pool(lambda e: e.tensor_tensor(out=xim[:], in0=tC[:], in1=tD[:], op=ALU.add), reads=[R_tC, R_tD], writes=[R_xim])

                def stF1(c):
                    t0 = c * 128
                    xre, R_xre = xreL[c % 2]
                    xim, R_xim = ximL[c % 2]
                    zg, R_zg = zgL[c % 2]
                    zgb, R_zgb = zgbL[c % 2]
                    for hf in range(2):
                        yreg = psb[7][:, hf * 128:(hf + 1) * 128]
                        first = True
                        for kk in range(4):
                            k = hf * 4 + kk
                            for ri, (xx, R_xx) in enumerate(((xre, R_xre), (xim, R_xim))):
                                P.pe(lambda e, k=k, ri=ri, xx=xx, yreg=yreg, first=first: e.matmul(
                                    yreg, lhsT=Ctab[:, 2 * k + ri, :], rhs=xx[:, k * 128:(k + 1) * 128], start=first, stop=False),
                                    reads=[R_Ctab, R_xx], writes=[R_ps[7]])
                                first = False
                        P.pe(lambda e, hf=hf, yreg=yreg: e.matmul(yreg, lhsT=Dtab[:, hf, :], rhs=suT[:, hf, t0:t0 + 128], start=False, stop=True),
                             reads=[R_Dtab, R_su], writes=[R_ps[7]])
                    yall = psb[7][:, 0:256].rearrange("p (h t) -> p h t", h=2)
                    P.act(lambda e: e.activation(out=sgt[:], in_=yall, func=AF.Square), reads=[R_ps[7]], writes=[R_sgt])
                    P.dve(lambda e: e.tensor_scalar(out=sgt[:], in0=sgt[:], scalar1=0.044715, scalar2=1.0, op0=ALU.mult, op1=ALU.add),
                          reads=[R_sgt], writes=[R_sgt])
                    P.dve(lambda e: e.tensor_tensor(out=sgt[:], in0=sgt[:], in1=yall, op=ALU.mult), reads=[R_sgt, R_ps[7]], writes=[R_sgt])
                    P.act(lambda e: e.activation(out=sgt[:], in_=sgt[:], func=AF.Tanh, scale=0.7978845608028654), reads=[R_sgt], writes=[R_sgt])
                    P.dve(lambda e: e.scalar_tensor_tensor(out=zg[:], in0=sgt[:], scalar=1.0, in1=yall, op0=ALU.add, op1=ALU.mult),
                          reads=[R_sgt, R_ps[7]], writes=[R_zg])
                    P.dve(lambda e: e.tensor_scalar(out=zgb[:], in0=zg[:], scalar1=0.5, scalar2=None, op0=ALU.mult), reads=[R_zg], writes=[R_zgb])

                def stF2(c):
                    zg, R_zg = zgL[c % 2]
                    zgb, R_zgb = zgbL[c % 2]
                    yo, R_yo = yoL[c % 2]
                    for hf in range(2):
                        greg = psb[7][:, 256 + hf * 128:256 + (hf + 1) * 128]
                        P.pe(lambda e, hf=hf, greg=greg: e.matmul(greg, lhsT=Gtab[:, hf, :], rhs=zgb[:, hf, :], start=True, stop=True),
                             reads=[R_Gtab, R_zgb], writes=[R_ps[7]])
                    for hf in range(2):
                        greg = psb[7][:, 256 + hf * 128:256 + (hf + 1) * 128]
                        P.act(lambda e, hf=hf, greg=greg: e.activation(out=th2[:, hf, :], in_=greg, func=AF.Tanh, scale=0.5, bias=dcol[:, hf, 3:4]),
                              reads=[R_ps[7], R_dcol], writes=[R_th2])
                    P.dve(lambda e: e.scalar_tensor_tensor(out=yo[:], in0=th2[:], scalar=1.0, in1=zg[:], op0=ALU.add, op1=ALU.mult),
                          reads=[R_th2, R_zg], writes=[R_yo])

                def stF3(c):
                    t0 = c * 128
                    yo, R_yo = yoL[c % 2]
                    P.act(lambda e: e.activation(out=ysq3[:], in_=yo[:], func=AF.Square, scale=0.25), reads=[R_yo], writes=[R_ysq3])
                    for hf in range(2):
                        P.pe(lambda e, hf=hf: e.matmul(psb[7][:, 256:257], lhsT=ysq3[:, hf, :], rhs=onescol[:], start=(hf == 0), stop=(hf == 1)),
                             reads=[R_ysq3, R_onescol], writes=[R_ps[7]])
                    P.dve(lambda e: e.tensor_copy(out=rsssm[:, c:c + 1], in_=psb[7][:, 256:257]), reads=[R_ps[7]], writes=[R_rsssm])
                    yb, R_yb = ybs[c % 2]
                    for hf in range(2):
                        P.act(lambda e, hf=hf: e.activation(out=yb[:, hf, :], in_=yo[:, hf, :], func=AF.Copy, scale=dcol[:, hf, 4:5]),
                              reads=[R_yo, R_dcol], writes=[R_yb])
                        P.dma("sp", mixT_d[768 + hf * 128:768 + (hf + 1) * 128, t0:t0 + 128], yb[:, hf, :], reads=[R_yb], writes=[R_mixs])

                s5_nsetup = len(P.ops)
                stages = (stA, stB, stC, stF1, stF2, stF3)
                for i in range(NT + len(stages) - 1):
                    for si, stg in enumerate(stages):
                        cc_ = i - si
                        if 0 <= cc_ < NT:
                            stg(cc_)
                s5_ops = P.side_end()
                run_attention(s5_ops)
                if debug:
                    P.dma("sp", rs_d[:, 0:NT * 8], rsatt[:].rearrange("p t h -> p (t h)"), reads=[R_rsatt])
                    P.dma("sp", rs_d[:, NT * 8:NT * 9], rsssm[:], reads=[R_rsssm])
                P.flush()
            if stop_after == ("S", l):
                break

            with contextlib.ExitStack() as st:
                last = (l == L - 1)
                wout, R_woutt = mk(st, "wout", (128, 8, D), BF16)
                w2all, R_w2all = mk(st, "w2all", (128, NE, 2, D), BF16)
                mixT, R_mixT = mk(st, "mixT", (128, 8, 512), BF16)
                hld = [mk(st, "hld%d" % i, (128, D), F32) for i in range(2)]
                hn, R_hn = mk(st, "hn", (128, 4, D), F32)
                tnL = [mk(st, "tn%d" % i, (128, D), F32) for i in range(2)]
                gfb, R_gfb = mk(st, "gfb", (128, D), F32)
                tTfL = [mk(st, "tTf%d" % i, (128, 8, 128), F32) for i in range(2)]
                tTb, R_tTb = mk(st, "tTb", (128, 8, 512), BF16)
                actg, R_actg = mk(st, "actg", (128, NE, 2, 512), BF16)
                w13 = [mk(st, "w13_%d" % i, (128, 8, 512), BF16) for i in range(2)]
                wr, R_wr = mk(st, "wr", (128, 8, 20), F32)
                rbias, R_rbias = mk(st, "rbias", (128, 20), F32)
                selE, R_selE = mk(st, "selE", (16, NE, 128), BF16)
                gTh, R_gTh = mk(st, "gTh", (16, 512), BF16)
                gTl, R_gTl = mk(st, "gTl", (16, 512), BF16)
                gtfL = [mk(st, "gtf%d" % i, (16, 128), F32) for i in range(2)]
                s1 = [mk(st, "s1_%d" % i, (128, 512), F32) for i in range(2)]
                r8L = [mk(st, "r8_%d" % i, (128, 16), F32) for i in range(2)]
                r8, R_r8 = r8L[0]
                lgL = [mk(st, "lg%d" % i, (128, 20), F32) for i in range(2)]
                rtL = [mk(st, "rt%d" % i, (128, 64), F32) for i in range(2)]
                gatesL = [mk(st, "gates%d" % i, (128, 16), F32) for i in range(2)]
                fgb, R_fgb = mk(st, "fgb", (128, D), F32)

                for kc in range(8):
                    P.dma("sp", wout[:, kc, :], wout_b[l, kc * 128:(kc + 1) * 128, :], reads=[R_wout[l]], writes=[R_woutt])
                P.dma("sp", gfb[:], prm["norm_ffn_g"][l:l + 1, :].partition_broadcast(128), writes=[R_gfb])
                if last:
                    P.dma("sp", fgb[:], prm["final_norm_g"].rearrange("(o d) -> o d", o=1).partition_broadcast(128), writes=[R_fgb])
                P.dma("sp", wr[:, :, 0:4], prm["router_g_w"][l].rearrange("(kc p) n -> p kc n", p=128), writes=[R_wr])
                P.dma("sp", wr[:, :, 4:20], prm["router_e_w"][l].rearrange("(kc p) n -> p kc n", p=128), writes=[R_wr])
                P.dma("sp", rbias[:, 0:4], prm["router_g_b"][l:l + 1, :].partition_broadcast(128), writes=[R_rbias])
                P.dma("sp", rbias[:, 4:20], prm["router_e_b"][l:l + 1, :].partition_broadcast(128), writes=[R_rbias])
                P.dve(lambda e: e.memset(selE[:], 0.0), writes=[R_selE])
                for e_ in range(NE):
                    P.dve(lambda e, e_=e_: e.tensor_scalar(out=selE[:, e_, :], in0=selE[:, e_, :], scalar1=ident[0:16, e_:e_ + 1], scalar2=None,
                                                           op0=ALU.add),
                          reads=[R_selE, R_ident], writes=[R_selE])
                pass

                def R8(i):
                    return r8[:, i:i + 1]

                wcnt = {"n": 0, "u": 0, "o": 0, "h": 0}
                def moe_block(t0, W):
                    ns = W // 128
                    P.dma("sp", mixT[:, :, 0:W], mixT_d[:, t0:t0 + W].rearrange("(kc p) t -> p kc t", p=128), reads=[R_mixc, R_mixa, R_mixs], writes=[R_mixT])
                    def d_sub(s, ch):
                        ti = t0 // 128 + s
                        hl, R_hl = hld[ch]
                        r8, R_r8 = r8L[ch]
                        lg, R_lg = lgL[ch]
                        rt, R_rt = rtL[ch]
                        gates, R_gates = gatesL[ch]
                        tn, R_tn = tnL[ch]
                        tTf, R_tTf = tTfL[ch]
                        gtf, R_gtf = gtfL[ch]
                        bk = [4 * ch + i for i in range(4)]

                        def R8(i):
                            return r8[:, i:i + 1]

                        load_h(l, hl, R_hl, ti)
                        P.dve(lambda e: e.tensor_reduce(out=R8(0), in_=rsatt[:, ti, :], axis=AX.X, op=ALU.add), reads=[R_rsatt], writes=[R_r8])
                        P.act(lambda e: e.activation(out=R8(1), in_=R8(0), func=AF.Sqrt, scale=1.0 / 512.0, bias=EPS), reads=[R_r8], writes=[R_r8])
                        P.act(lambda e: e.activation(out=R8(2), in_=rsssm[:, ti:ti + 1], func=AF.Sqrt, scale=1.0 / 256.0, bias=EPS),
                              reads=[R_rsssm], writes=[R_r8])
                        P.dve(lambda e: e.reciprocal(out=r8[:, 3:5], in_=r8[:, 1:3]), reads=[R_r8], writes=[R_r8])
                        yield
                        for gi, kcs in enumerate(((0, 1), (2, 3, 4, 5), (6, 7))):
                            for nh in range(2):
                                bank = bk[(2 * gi + nh) % 4]
                                for j, kc in enumerate(kcs):
                                    P.pe(lambda e, kc=kc, nh=nh, bank=bank, j=j, n=len(kcs): e.matmul(
                                        psb[bank][:, :], lhsT=mixT[:, kc, s * 128:(s + 1) * 128], rhs=wout[:, kc, nh * 512:(nh + 1) * 512],
                                        start=(j == 0), stop=(j == n - 1)),
                                        reads=[R_mixT, R_woutt], writes=[R_ps[bank]])
                            yield
                            for nh in range(2):
                                bank = bk[(2 * gi + nh) % 4]
                                cs = slice(nh * 512, (nh + 1) * 512)
                                if gi == 0:
                                    P.dve(lambda e, cs=cs, bank=bank: e.tensor_tensor(out=hn[:, s, cs], in0=psb[bank][:], in1=hl[:, cs], op=ALU.add),
                                          reads=[R_ps[bank], R_hl], writes=[R_hn])
                                else:
                                    P.dve(lambda e, cs=cs, bank=bank, gi=gi: e.scalar_tensor_tensor(out=hn[:, s, cs], in0=psb[bank][:], scalar=R8(2 + gi),
                                                                                                    in1=hn[:, s, cs], op0=ALU.mult, op1=ALU.add),
                                          reads=[R_ps[bank], R_r8, R_hn], writes=[R_hn])
                            yield
                        P.act(lambda e: e.activation(out=tn[:], in_=hn[:, s, :], func=AF.Square, accum_out=R8(5)), reads=[R_hn], writes=[R_tn, R_r8])
                        P.act(lambda e: e.activation(out=R8(6), in_=R8(5), func=AF.Sqrt, scale=1.0 / D, bias=EPS), reads=[R_r8], writes=[R_r8])
                        yield
                        P.dve(lambda e: e.reciprocal(out=R8(7), in_=R8(6)), reads=[R_r8], writes=[R_r8])
                        P.dve(lambda e: e.scalar_tensor_tensor(out=tn[:], in0=hn[:, s, :], scalar=R8(7), in1=gfb[:], op0=ALU.mult, op1=ALU.mult),
                              reads=[R_hn, R_r8, R_gfb], writes=[R_tn])
                        yield
                        for half in range(2):
                            bank = bk[2 + half]
                            for c4 in range(4):
                                kc = half * 4 + c4
                                P.pe(lambda e, kc=kc, c4=c4, bank=bank: e.transpose(psb[bank][:, c4 * 128:(c4 + 1) * 128],
                                                                                   tn[:, kc * 128:(kc + 1) * 128], ident[:]),
                                     reads=[R_tn, R_ident], writes=[R_ps[bank]])
                        yield
                        P.act(lambda e: e.copy(out=tTf[:, 0:4, :], in_=psb[bk[2]][:].rearrange("p (c t) -> p c t", c=4)), reads=[R_ps[bk[2]]], writes=[R_tTf])
                        P.dve(lambda e: e.tensor_copy(out=tTf[:, 4:8, :], in_=psb[bk[3]][:].rearrange("p (c t) -> p c t", c=4)), reads=[R_ps[bk[3]]], writes=[R_tTf])
                        yield
                        for kc in range(8):
                            P.pe(lambda e, kc=kc: e.matmul(psb[bk[0]][:, 0:20], lhsT=tTf[:, kc, :], rhs=wr[:, kc, :], start=(kc == 0), stop=(kc == 7)),
                                 reads=[R_tTf, R_wr], writes=[R_ps[bk[0]]])
                        P.act(lambda e: e.copy(out=tTb[:, :, s * 128:(s + 1) * 128], in_=tTf[:]), reads=[R_tTf], writes=[R_tTb])
                        yield
                        P.dve(lambda e: e.tensor_tensor(out=lg[:], in0=psb[bk[0]][:, 0:20], in1=rbias[:], op=ALU.add), reads=[R_ps[bk[0]], R_rbias], writes=[R_lg])
                        RW = [R_rt, R_r8, R_lg]
                        P.dve(lambda e: e.tensor_reduce(out=R8(8), in_=lg[:, 0:4], axis=AX.X, op=ALU.max), reads=RW, writes=RW)
                        P.dve(lambda e: e.tensor_scalar(out=rt[:, 0:4], in0=lg[:, 0:4], scalar1=R8(8), scalar2=None, op0=ALU.is_equal), reads=RW, writes=RW)
                        P.dve(lambda e: e.tensor_scalar(out=R8(9), in0=R8(8), scalar1=-1.0, scalar2=None, op0=ALU.mult), reads=RW, writes=RW)
                        yield
                        P.act(lambda e: e.activation(out=rt[:, 24:28], in_=lg[:, 0:4], func=AF.Exp, bias=R8(9), accum_out=R8(10)), reads=RW, writes=RW)
                        yield
                        P.dve(lambda e: e.reciprocal(out=R8(11), in_=R8(10)), reads=RW, writes=RW)
                        P.dve(lambda e: e.tensor_scalar(out=rt[:, 4:8], in0=lg[:, 4:8], scalar1=rt[:, 0:1], scalar2=None, op0=ALU.mult), reads=RW, writes=RW)
                        for g in range(1, 4):
                            P.dve(lambda e, g=g: e.scalar_tensor_tensor(out=rt[:, 4:8], in0=lg[:, 4 + 4 * g:8 + 4 * g], scalar=rt[:, g:g + 1], in1=rt[:, 4:8],
                                                                        op0=ALU.mult, op1=ALU.add), reads=RW, writes=RW)
                        P.dve(lambda e: e.tensor_reduce(out=R8(12), in_=rt[:, 4:8], axis=AX.X, op=ALU.max), reads=RW, writes=RW)
                        P.dve(lambda e: e.tensor_scalar(out=rt[:, 8:12], in0=rt[:, 4:8], scalar1=R8(12), scalar2=None, op0=ALU.is_equal), reads=RW, writes=RW)
                        P.dve(lambda e: e.scalar_tensor_tensor(out=rt[:, 12:16], in0=rt[:, 8:12], scalar=-1e30, in1=rt[:, 4:8], op0=ALU.mult, op1=ALU.add),
                              reads=RW, writes=RW)
                        P.dve(lambda e: e.tensor_reduce(out=R8(13), in_=rt[:, 12:16], axis=AX.X, op=ALU.max), reads=RW, writes=RW)
                        P.dve(lambda e: e.tensor_scalar(out=rt[:, 16:20], in0=rt[:, 12:16], scalar1=R8(13), scalar2=None, op0=ALU.is_equal), reads=RW, writes=RW)
                        P.dve(lambda e: e.tensor_tensor(out=R8(14), in0=R8(13), in1=R8(12), op=ALU.subtract), reads=RW, writes=RW)
                        yield
                        P.act(lambda e: e.activation(out=R8(14), in_=R8(14), func=AF.Exp), reads=RW, writes=RW)
                        yield
                        P.dve(lambda e: e.tensor_scalar(out=R8(15), in0=R8(14), scalar1=1.0, scalar2=None, op0=ALU.add), reads=RW, writes=RW)
                        P.dve(lambda e: e.reciprocal(out=R8(15), in_=R8(15)), reads=RW, writes=RW)
                        P.dve(lambda e: e.tensor_tensor(out=R8(15), in0=R8(15), in1=R8(11), op=ALU.mult), reads=RW, writes=RW)
                        P.dve(lambda e: e.tensor_tensor(out=R8(14), in0=R8(14), in1=R8(15), op=ALU.mult), reads=RW, writes=RW)
                        P.dve(lambda e: e.tensor_scalar(out=rt[:, 20:24], in0=rt[:, 8:12], scalar1=R8(15), scalar2=None, op0=ALU.mult), reads=RW, writes=RW)
                        P.dve(lambda e: e.scalar_tensor_tensor(out=rt[:, 20:24], in0=rt[:, 16:20], scalar=R8(14), in1=rt[:, 20:24], op0=ALU.mult, op1=ALU.add),
                              reads=RW, writes=RW)
                        for g in range(4):
                            P.dve(lambda e, g=g: e.tensor_scalar(out=gates[:, 4 * g:4 * g + 4], in0=rt[:, 20:24], scalar1=rt[:, g:g + 1], scalar2=None,
                                                                 op0=ALU.mult), reads=RW, writes=[R_gates])
                        yield
                        P.pe(lambda e: e.transpose(psb[bk[1]][0:16, 0:128], gates[:], ident[:]), reads=[R_gates, R_ident], writes=[R_ps[bk[1]]])
                        yield
                        P.act(lambda e: e.copy(out=gTh[:, s * 128:(s + 1) * 128], in_=psb[bk[1]][0:16, 0:128]), reads=[R_ps[bk[1]]], writes=[R_gTh])
                        P.dve(lambda e: e.tensor_tensor(out=gtf[:], in0=psb[bk[1]][0:16, 0:128], in1=gTh[:, s * 128:(s + 1) * 128], op=ALU.subtract),
                              reads=[R_ps[bk[1]], R_gTh], writes=[R_gtf])
                        P.dve(lambda e: e.tensor_copy(out=gTl[:, s * 128:(s + 1) * 128], in_=gtf[:]), reads=[R_gtf], writes=[R_gTl])

                    for s0 in range(0, ns, 2):
                        gens = [d_sub(s0, 0)] + ([d_sub(s0 + 1, 1)] if s0 + 1 < ns else [])
                        while gens:
                            for g_ in list(gens):
                                try:
                                    next(g_)
                                except StopIteration:
                                    gens.remove(g_)
                    if t0 == 0:
                        for e_ in range(NE):
                            P.dma("sp", w2all[:, e_, :, :], w2_b[l, e_].rearrange("(fh p) n -> p fh n", p=128), reads=[R_w2[l]], writes=[R_w2all])
                    for e_ in range(NE):
                        wt, R_wt = w13[wcnt["n"] % 2]
                        wcnt["n"] += 1
                        P.dma("sp", wt[:], w13_b[l, e_].rearrange("(kc p) f -> p kc f", p=128), reads=[R_w13[l]], writes=[R_wt])
                        gbank = 4 + e_ % 2
                        P.pe(lambda e, e_=e_, gbank=gbank: e.matmul(psb[gbank][:, 0:W], lhsT=selE[:, e_, :], rhs=gTh[:, 0:W], start=True, stop=False),
                             reads=[R_selE, R_gTh], writes=[R_ps[gbank]])
                        P.pe(lambda e, e_=e_, gbank=gbank: e.matmul(psb[gbank][:, 0:W], lhsT=selE[:, e_, :], rhs=gTl[:, 0:W], start=False, stop=True),
                             reads=[R_selE, R_gTl], writes=[R_ps[gbank]])
                        for fh in range(2):
                            u = wcnt["u"]
                            wcnt["u"] += 1
                            b1, b3 = 2 * (u % 2), 2 * (u % 2) + 1
                            for kc in range(8):
                                P.pe(lambda e, kc=kc, fh=fh, wt=wt, b1=b1: e.matmul(psb[b1][:, 0:W], lhsT=wt[:, kc, fh * 128:(fh + 1) * 128],
                                                                                   rhs=tTb[:, kc, 0:W], start=(kc == 0), stop=(kc == 7)),
                                     reads=[R_wt, R_tTb], writes=[R_ps[b1]])
                            for kc in range(8):
                                P.pe(lambda e, kc=kc, fh=fh, wt=wt, b3=b3: e.matmul(psb[b3][:, 0:W], lhsT=wt[:, kc, 256 + fh * 128:256 + (fh + 1) * 128],
                                                                                   rhs=tTb[:, kc, 0:W], start=(kc == 0), stop=(kc == 7)),
                                     reads=[R_wt, R_tTb], writes=[R_ps[b3]])
                            s1t, R_s1 = s1[u % 2]
                            P.act(lambda e, s1t=s1t, b1=b1: e.activation(out=s1t[:, 0:W], in_=psb[b1][:, 0:W], func=AF.Silu),
                                  reads=[R_ps[b1]], writes=[R_s1])
                            P.dve(lambda e, s1t=s1t, b3=b3: e.tensor_tensor(out=s1t[:, 0:W], in0=s1t[:, 0:W], in1=psb[b3][:, 0:W], op=ALU.mult),
                                  reads=[R_s1, R_ps[b3]], writes=[R_s1])
                            P.dve(lambda e, s1t=s1t, e_=e_, fh=fh, gbank=gbank: e.tensor_tensor(out=actg[:, e_, fh, 0:W], in0=s1t[:, 0:W],
                                                                                               in1=psb[gbank][:, 0:W], op=ALU.mult),
                                  reads=[R_s1, R_ps[gbank]], writes=[R_actg])
                    for s in range(ns):
                        ti = t0 // 128 + s
                        hot, R_hot = hn[:, s, :], R_hn
                        for nh in range(2):
                            bank = 6 + nh
                            n = 0
                            for e_ in range(NE):
                                for fh in range(2):
                                    P.pe(lambda e, e_=e_, fh=fh, nh=nh, bank=bank, n=n, s=s: e.matmul(
                                        psb[bank][:, :], lhsT=actg[:, e_, fh, s * 128:(s + 1) * 128], rhs=w2all[:, e_, fh, nh * 512:(nh + 1) * 512],
                                        start=(n == 0), stop=(n == 2 * NE - 1)),
                                        reads=[R_actg, R_w2all], writes=[R_ps[bank]])
                                    n += 1
                            cs = slice(nh * 512, (nh + 1) * 512)
                            P.dve(lambda e, nh=nh, cs=cs, bank=bank, s=s: e.tensor_tensor(out=hn[:, s, cs], in0=psb[bank][:], in1=hn[:, s, cs], op=ALU.add),
                                  reads=[R_ps[bank], R_hn], writes=[R_hn])
                        if not last:
                            P.dma("pool", h_d[ti * 128:(ti + 1) * 128, :], hot, reads=[R_hot], writes=[R_h[ti]])
                        else:
                            if debug:
                                P.dma("pool", h_d[ti * 128:(ti + 1) * 128, :], hot, reads=[R_hot], writes=[R_h[ti]])
                            P.act(lambda e, hot=hot: e.activation(out=tnL[0][0][:], in_=hot, func=AF.Square, accum_out=R8(5)),
                                  reads=[R_hot], writes=[tnL[0][1], R_r8])
                            P.act(lambda e: e.activation(out=R8(6), in_=R8(5), func=AF.Sqrt, scale=1.0 / D, bias=EPS), reads=[R_r8], writes=[R_r8])
                            P.dve(lambda e: e.reciprocal(out=R8(7), in_=R8(6)), reads=[R_r8], writes=[R_r8])
                            P.dve(lambda e, hot=hot: e.scalar_tensor_tensor(out=hot, in0=hot, scalar=R8(7), in1=fgb[:], op0=ALU.mult, op1=ALU.mult),
                                  reads=[R_hot, R_r8, R_fgb], writes=[R_hot])
                            lo = max(ti * 128, 16)
                            hi = min((ti + 1) * 128, T)
                            if hi > lo:
                                P.dma("pool", out[lo - 16:hi - 16, :], hn[lo - ti * 128:hi - ti * 128, s, :], reads=[R_hot])
                for (t0, W) in blocks:
                    moe_block(t0, W)
                P.flush()
        if stop_after is not None:
            P.flush()
        stats = dict(nops=P.nop_total, nwait=P.nwait, cnt=dict(P.cnt), dcnt=dict(P.dcnt))
    return nc, stats


_CACHE = {}


def kernel(**inputs):
    x = np.ascontiguousarray(inputs["x"], dtype=np.float32)
    B, SEQ, _ = x.shape
    key = (SEQ,)
    if key not in _CACHE:
        _CACHE[key] = build(SEQ)
    nc, _ = _CACHE[key]
    consts = host_consts()
    in_maps = []
    for b in range(B):
        m = {"x": x[b]}
        for k in PARAM_SHAPES:
            m[k] = np.ascontiguousarray(inputs[k], dtype=np.float32)
        m.update(consts)
        in_maps.append(m)
    res = run_bass_kernel_spmd(nc, in_maps, core_ids=list(range(B)))
    return np.stack([np.asarray(r["out"], dtype=np.float32) for r in res.results], axis=0)
```

```python
import contextlib
import math
import numpy as np
import concourse.bass as bass
import concourse.mybir as mybir
from concourse.bass_utils import run_bass_kernel_spmd

F32 = mybir.dt.float32
BF16 = mybir.dt.bfloat16
I32 = mybir.dt.int32
AF = mybir.ActivationFunctionType
ALU = mybir.AluOpType
AX = mybir.AxisListType

D = 1024
DIN = 2312
NH = 8
EPS = 1e-6
CW = 31
NE = 16
PI = math.pi


class Res:
    __slots__ = ("name", "last_w", "readers")

    def __init__(self, name=""):
        self.name = name
        self.last_w = None
        self.readers = []


class Op:
    __slots__ = ("eng", "fn", "deps", "sig", "sigval", "dma", "dsem", "dval", "idx")


class Prog:
    ENGS = ("pe", "act", "dve", "pool", "sp")
    QS = ("sp", "pool", "act")

    def __init__(self, nc, stack, ring=16):
        self.nc = nc
        self.ops = []
        self.ring = ring
        self.handles = {"pe": nc.tensor, "act": nc.scalar, "dve": nc.vector,
                        "pool": nc.gpsimd, "sp": nc.sync}
        self.esem = {e: stack.enter_context(nc.semaphore("s_" + e)) for e in self.ENGS}
        self.rings = {q: [stack.enter_context(nc.semaphore("r_%s%d" % (q, i))) for i in range(ring)]
                      for q in self.QS}
        self.cnt = {e: 0 for e in self.ENGS}
        self.dcnt = {q: 0 for q in self.QS}
        self.seen = {e: {} for e in self.ENGS}
        self.touched = set()
        self.nop_total = 0
        self.nwait = 0

    def op(self, eng, fn, reads=(), writes=(), dma=False):
        o = Op()
        o.eng = eng
        o.fn = fn
        o.dma = dma
        o.sig = dma
        o.sigval = 0
        o.idx = len(self.ops)
        deps = {}
        for r in reads:
            if r.last_w is not None:
                deps[id(r.last_w)] = r.last_w
        for w in writes:
            if w.last_w is not None:
                deps[id(w.last_w)] = w.last_w
            for rd in w.readers:
                deps[id(rd)] = rd
        dl = []
        for d in deps.values():
            if d is o:
                continue
            if eng == "pe" and d.eng == "pe" and not d.dma:
                continue
            d.sig = True
            dl.append(d)
        o.deps = dl
        for r in reads:
            r.readers.append(o)
            self.touched.add(r)
        for w in writes:
            w.last_w = o
            w.readers = []
            self.touched.add(w)
        self.ops.append(o)
        return o

    def pe(self, fn, reads=(), writes=()):
        return self.op("pe", fn, reads, writes)

    def act(self, fn, reads=(), writes=()):
        return self.op("act", fn, reads, writes)

    def dve(self, fn, reads=(), writes=()):
        return self.op("dve", fn, reads, writes)

    def pool(self, fn, reads=(), writes=()):
        return self.op("pool", fn, reads, writes)

    def dma(self, q, out, in_, reads=(), writes=(), **kw):
        return self.op(q, lambda e: e.dma_start(out=out, in_=in_, allow_slow_non_contiguous=True, **kw),
                       reads, writes, dma=True)

    def side_begin(self):
        self._saved = self.ops
        self.ops = []

    def side_end(self):
        side = self.ops
        self.ops = self._saved
        return side

    def flush(self):
        ring = self.ring
        last = {}
        for o in self.ops:
            if not o.dma:
                last[o.eng] = o
        for o in last.values():
            o.sig = True
        for o in self.ops:
            if o.dma:
                j = self.dcnt[o.eng]
                self.dcnt[o.eng] += 1
                o.dsem = (o.eng, j % ring)
                o.dval = 16 * (j // ring + 1)
            elif o.sig:
                self.cnt[o.eng] += 1
                o.sigval = self.cnt[o.eng]
        for o in self.ops:
            h = self.handles[o.eng]
            sn = self.seen[o.eng]
            if o.dma:
                q, slot = o.dsem
                prev = o.dval - 16
                if prev > 0 and sn.get(("r", q, slot), 0) < prev:
                    h.wait_ge(self.rings[q][slot], prev)
                    sn[("r", q, slot)] = prev
                    self.nwait += 1
            for d in o.deps:
                if d.dma:
                    key = ("r",) + d.dsem
                    val = d.dval
                    sem = self.rings[d.dsem[0]][d.dsem[1]]
                else:
                    key = ("e", d.eng)
                    val = d.sigval
                    sem = self.esem[d.eng]
                if sn.get(key, 0) >= val:
                    continue
                h.wait_ge(sem, val)
                sn[key] = val
                self.nwait += 1
            ins = o.fn(h)
            if o.dma:
                ins.then_inc(self.rings[o.dsem[0]][o.dsem[1]], 16)
            elif o.sig:
                ins.then_inc(self.esem[o.eng], 1)
        self.nop_total += len(self.ops)
        self.ops = []
        sp = self.handles["sp"]
        sn = self.seen["sp"]
        for e in self.ENGS:
            if e == "sp":
                continue
            v = self.cnt[e]
            if v > 0 and sn.get(("e", e), 0) < v:
                sp.wait_ge(self.esem[e], v)
                sn[("e", e)] = v
        for q in self.QS:
            n = self.dcnt[q]
            for slot in range(ring):
                uses = (n - slot + ring - 1) // ring if n > slot else 0
                v = 16 * uses
                if v > 0 and sn.get(("r", q, slot), 0) < v:
                    sp.wait_ge(self.rings[q][slot], v)
                    sn[("r", q, slot)] = v
        sp.sem_inc(self.esem["sp"], 1)
        self.cnt["sp"] += 1
        bv = self.cnt["sp"]
        for e in self.ENGS:
            s2 = self.seen[e]
            if e != "sp":
                self.handles[e].wait_ge(self.esem["sp"], bv)
            for e2 in self.ENGS:
                s2[("e", e2)] = self.cnt[e2]
            for q in self.QS:
                n = self.dcnt[q]
                for slot in range(ring):
                    uses = (n - slot + ring - 1) // ring if n > slot else 0
                    s2[("r", q, slot)] = 16 * uses
        for r in self.touched:
            r.last_w = None
            r.readers = []
        self.touched = set()


PARAM_SHAPES = {
    "meta_tokens": (16, 1024), "norm_mix_g": (2, 1024), "w_in": (2, 1024, 2312), "fgate_b": (2, 8),
    "conv_w": (2, 31, 256), "conv_b": (2, 256), "conv_ln_g": (2, 256), "conv_ln_b": (2, 256),
    "att_norm_g": (2, 512), "ssm_lam_re": (2, 16, 64), "ssm_lam_im": (2, 16, 64), "ssm_log_dt": (2, 16),
    "ssm_b_re": (2, 16, 64, 16), "ssm_b_im": (2, 16, 64, 16), "ssm_c_re": (2, 16, 16, 64),
    "ssm_c_im": (2, 16, 16, 64), "ssm_d": (2, 256), "ssm_glu_w": (2, 16, 16, 16), "ssm_glu_b": (2, 16, 16),
    "ssm_norm_g": (2, 256), "w_out": (2, 1024, 1024), "norm_ffn_g": (2, 1024), "router_g_w": (2, 1024, 4),
    "router_g_b": (2, 4), "router_e_w": (2, 1024, 16), "router_e_b": (2, 16),
    "exp_w1": (2, 16, 1024, 256), "exp_w3": (2, 16, 1024, 256), "exp_w2": (2, 16, 256, 1024),
    "final_norm_g": (1024,),
}


def host_consts():
    ident = np.eye(128, dtype=np.float32)
    p = np.arange(128)[:, None]
    f = np.arange(128)[None, :]
    maskneg = np.where(p <= f, 0.0, -30000.0).astype(np.float32)
    iota = np.broadcast_to(np.arange(128, dtype=np.float32)[None, :], (128, 128)).copy()
    onesm = np.ones((128, 128), np.float32)
    onesm[:, 0] = 0.0
    return {"c_ident": ident, "c_maskneg": maskneg, "c_iota": iota, "c_onesm": onesm}


def build(SEQ, L=2, debug=False, stop_after=None):
    T = SEQ + 16
    NT = (T + 127) // 128
    TP = NT * 128
    blocks = [(t0, min(512, TP - t0)) for t0 in range(0, TP, 512)]
    nc = bass.Bass("TRN2", target_bir_lowering=False)
    dbgkind = "ExternalOutput" if debug else "Internal"

    def din(name, shape, dt=F32):
        return nc.dram_tensor(name, list(shape), dt, kind="ExternalInput").ap()

    def dscr(name, shape, dt, dbg=False):
        if dbg and debug:
            return nc.dram_tensor(name, list(shape), dt, kind="ExternalOutput").ap()
        return nc.dram_tensor(name, list(shape), dt).ap()

    x = din("x", (SEQ, D))
    prm = {k: din(k, v) for k, v in PARAM_SHAPES.items()}
    cst = {k: din(k, (128, 128)) for k in ("c_ident", "c_maskneg", "c_iota", "c_onesm")}
    out = nc.dram_tensor("out", [SEQ, D], F32, kind="ExternalOutput").ap()

    h_d = dscr("h_d", (TP, D), F32, dbg=True)
    qT_d = dscr("qT_d", (NH, 70, TP), BF16, dbg=True)
    kT_d = dscr("kT_d", (NH, 70, TP), BF16, dbg=True)
    v_d = dscr("v_d", (NH, TP, 64), BF16, dbg=True)
    mixT_d = dscr("mixT_d", (D, TP), BF16, dbg=True)
    rs_d = dscr("rs_d", (128, NT * 9), F32, dbg=True)
    suT_d = dscr("suT_d", (256, TP), BF16)
    win_b = dscr("win_b", (L, D, DIN), BF16)
    wout_b = dscr("wout_b", (L, D, D), BF16)
    w13_b = dscr("w13_b", (L, NE, D, 512), BF16)
    w2_b = dscr("w2_b", (L, NE, 256, D), BF16)

    with contextlib.ExitStack() as gst:
        P = Prog(nc, gst)
        R_h = [Res("h%d" % i) for i in range(NT)]
        R_q, R_k, R_v, R_mix = Res("qT"), Res("kT"), Res("vd"), Res("mix")
        R_sud = Res("sud")
        R_mixc, R_mixa, R_mixs = Res("mixc"), Res("mixa"), Res("mixs")
        R_win = [Res("win%d" % l) for l in range(L)]
        R_wout = [Res("wout%d" % l) for l in range(L)]
        R_w13 = [Res("w13_%d" % l) for l in range(L)]
        R_w2 = [Res("w2_%d" % l) for l in range(L)]

        uid = {"n": 0}

        def mk(st, name, shape, dt):
            uid["n"] += 1
            return st.enter_context(nc.sbuf_tensor("%s_%d" % (name, uid["n"]), list(shape), dt)), Res(name)

        psb = [gst.enter_context(nc.psum_tensor("psb%d" % i, [128, 512], F32)) for i in range(8)]
        R_ps = [Res("ps%d" % i) for i in range(8)]
        R_ps7 = [R_ps[7]] * 4
        ident, R_ident = mk(gst, "ident", (128, 128), F32)
        rsatt, R_rsatt = mk(gst, "rsatt", (128, NT, NH), F32)
        rsssm, R_rsssm = mk(gst, "rsssm", (128, NT), F32)
        onescol, R_onescol = mk(gst, "onescol", (128, 1), F32)

        def cast_layer_small(l):
            for kc in range(8):
                P.dma("pool", win_b[l, kc * 128:(kc + 1) * 128, :], prm["w_in"][l, kc * 128:(kc + 1) * 128, :],
                      writes=[R_win[l]])

        def cast_layer_big(l):
            for kc in range(4):
                P.dma("pool", wout_b[l, kc * 256:(kc + 1) * 256, :], prm["w_out"][l, kc * 256:(kc + 1) * 256, :],
                      writes=[R_wout[l]])
            for e in range(NE):
                P.dma("pool", w13_b[l, e, :, 0:256], prm["exp_w1"][l, e], writes=[R_w13[l]])
                P.dma("pool", w13_b[l, e, :, 256:512], prm["exp_w3"][l, e], writes=[R_w13[l]])
                P.dma("pool", w2_b[l, e], prm["exp_w2"][l, e], writes=[R_w2[l]])

        with contextlib.ExitStack() as st:
            zt, R_zt = mk(st, "zt", (128, D), F32)
            onesb, R_onesb = mk(st, "onesb", (NH, 3, TP), BF16)
            P.dma("sp", ident[:], cst["c_ident"], writes=[R_ident])
            P.dve(lambda e: e.memset(zt[:], 0.0), writes=[R_zt])
            P.dve(lambda e: e.memset(onesb[:], 1.0), writes=[R_onesb])
            P.dve(lambda e: e.memset(onescol[:], 1.0), writes=[R_onescol])
            if TP > T:
                P.dma("sp", h_d[T:TP, :], zt[0:TP - T, :], reads=[R_zt], writes=R_h)
            P.dma("sp", qT_d[:, 67:70, :], onesb[:], reads=[R_onesb], writes=[R_q])
            P.dma("sp", kT_d[:, 64:67, :], onesb[:], reads=[R_onesb], writes=[R_k])
            P.flush()

        def load_h(l, dst, R_dst, ti):
            if l > 0:
                P.dma("sp", dst[:], h_d[ti * 128:(ti + 1) * 128, :], reads=[R_h[ti]], writes=[R_dst])
                return
            lo, hi = ti * 128, (ti + 1) * 128
            if ti == 0:
                P.dma("sp", dst[0:16, :], prm["meta_tokens"], writes=[R_dst])
                P.dma("sp", dst[16:128, :], x[0:112, :], writes=[R_dst])
            elif hi <= T:
                P.dma("sp", dst[:], x[lo - 16:hi - 16, :], writes=[R_dst])
            else:
                nv = T - lo
                P.dma("sp", dst[0:nv, :], x[lo - 16:T - 16, :], writes=[R_dst])
                P.dma("sp", dst[nv:128, :], h_d[T:TP, :], reads=[R_h[ti]], writes=[R_dst])

        for l in range(L):
            with contextlib.ExitStack() as st:
                stq_a = "pool" if l > 0 else "sp"
                win, R_wint = mk(st, "win", (128, 8, DIN), BF16)
                gb, R_gb = mk(st, "gb", (128, D), F32)
                hbuf = [mk(st, "hbuf%d" % i, (128, D), F32) for i in range(2)]
                junk, R_junk = mk(st, "junk", (128, D), BF16)
                znb = [mk(st, "znb%d" % i, (128, D), F32) for i in range(2)]
                zT = [mk(st, "zT%d" % i, (128, 8, 512), BF16) for i in range(2)]
                st8 = [mk(st, "st8_%d" % i, (128, 8), F32) for i in range(2)]
                uT, R_uT = mk(st, "uT", (128, 2, TP + 32), BF16)
                sg, R_sg = mk(st, "sg", (128, 512), F32)
                qkst = [mk(st, "qkst%d" % i, (128, 512), BF16) for i in range(2)]
                vst = [mk(st, "vst%d" % i, (128, 512), BF16) for i in range(2)]
                negb, R_negb = mk(st, "negb", (NH, 1), F32)
                fe, R_fe = mk(st, "fe", (NH, 512), F32)
                ones8, R_ones8 = mk(st, "ones8", (NH, 512), F32)
                crow, R_crow = mk(st, "crow", (NH, TP), F32)
                cres, R_cres = mk(st, "cres", (NH, 512), F32)
                cpos, R_cpos = mk(st, "cpos", (NH, 3, 512), BF16)
                cneg, R_cneg = mk(st, "cneg", (NH, 3, 512), BF16)
                cwl, R_cwl = mk(st, "cwl", (CW, 256), F32)
                cwT, R_cwT = mk(st, "cwT", (128, 2, 32), F32)
                diagw, R_diagw = mk(st, "diagw", (128, 2, CW, 128), BF16)
                cvp, R_cvp = mk(st, "cvp", (128, 2, 4), F32)
                ycv, R_ycv = mk(st, "ycv", (128, 2, 512), F32)
                ysq, R_ysq = mk(st, "ysq", (128, 2, 512), F32)
                onesf, R_onesf = mk(st, "onesf", (128, 128), F32)
                mean, R_mean = mk(st, "mean", (128, 512), F32)
                var, R_var = mk(st, "var", (128, 512), F32)
                rstd, R_rstd = mk(st, "rstd", (128, 512), F32)
                cvo = [mk(st, "cvo%d" % i, (128, 512), BF16) for i in range(2)]
                sust = [mk(st, "sust%d" % i, (128, 512), BF16) for i in range(2)]

                if l == 0:
                    wstg = [mk(st, "wstg%d" % i, (128, DIN), F32) for i in range(2)]
                for kc in range(8):
                    if l == 0:
                        ws_, R_ws = wstg[kc % 2]
                        P.dma("sp", ws_[:], prm["w_in"][l, kc * 128:(kc + 1) * 128, :], writes=[R_ws])
                        if kc % 2 == 0:
                            P.dve(lambda e, ws_=ws_, kc=kc: e.tensor_copy(out=win[:, kc, :], in_=ws_[:]), reads=[R_ws], writes=[R_wint])
                        else:
                            P.act(lambda e, ws_=ws_, kc=kc: e.copy(out=win[:, kc, :], in_=ws_[:]), reads=[R_ws], writes=[R_wint])
                    else:
                        P.dma("sp", win[:, kc, :], win_b[l, kc * 128:(kc + 1) * 128, :], reads=[R_win[l]], writes=[R_wint])
                P.dma("sp", gb[:], prm["norm_mix_g"][l:l + 1, :].partition_broadcast(128), writes=[R_gb])
                P.dma("sp", negb[:], prm["fgate_b"][l:l + 1, :].rearrange("o h -> h o"), writes=[R_negb])
                P.act(lambda e: e.mul(out=negb[:], in_=negb[:], mul=-1.0), reads=[R_negb], writes=[R_negb])
                P.dve(lambda e: e.memset(ones8[:], 1.0), writes=[R_ones8])
                P.dve(lambda e: e.memset(onesf[:], 1.0 / 256.0), writes=[R_onesf])
                P.dve(lambda e: e.memset(uT[:, :, 0:32], 0.0), writes=[R_uT])
                P.dma("sp", cwl[:], prm["conv_w"][l], writes=[R_cwl])
                for hf in range(2):
                    P.pe(lambda e, hf=hf: e.transpose(psb[7][:, 0:CW], cwl[:, hf * 128:(hf + 1) * 128], ident[0:CW, 0:CW]),
                         reads=[R_cwl, R_ident], writes=[R_ps[7]])
                    P.dve(lambda e, hf=hf: e.tensor_copy(out=cwT[:, hf, 0:CW], in_=psb[7][:, 0:CW]),
                          reads=[R_ps[7]], writes=[R_cwT])
                for hf in range(2):
                    for j in range(CW):
                        P.dve(lambda e, hf=hf, j=j: e.tensor_scalar(out=diagw[:, hf, j, :], in0=ident[:],
                                                                    scalar1=cwT[:, hf, j:j + 1], scalar2=None, op0=ALU.mult),
                              reads=[R_ident, R_cwT], writes=[R_diagw])
                for i, nm in enumerate(("conv_b", "conv_ln_g", "conv_ln_b")):
                    P.dma("sp", cvp[:, :, i], prm[nm][l].rearrange("(hf p) -> p hf", p=128), writes=[R_cvp])
                P.flush()

                if l == 0:
                    cast_layer_big(0)
                cstate = {"n": 0}

                def prep(b):
                    t0, W = blocks[b]
                    zt_, R_z = zT[b % 2]
                    for s in range(W // 128):
                        i = cstate["n"]
                        cstate["n"] += 1
                        ht, R_ht = hbuf[i % 2]
                        zn, R_zn = znb[i % 2]
                        s8, R_s8 = st8[i % 2]
                        ti = (t0 + s * 128) // 128
                        load_h(l, ht, R_ht, ti)
                        P.act(lambda e, ht=ht, s8=s8: e.activation(out=junk[:], in_=ht[:], func=AF.Square, accum_out=s8[:, 0:1]),
                              reads=[R_ht], writes=[R_junk, R_s8])
                        P.act(lambda e, s8=s8: e.activation(out=s8[:, 1:2], in_=s8[:, 0:1], func=AF.Sqrt, scale=1.0 / D, bias=EPS),
                              reads=[R_s8], writes=[R_s8])
                        P.dve(lambda e, s8=s8: e.reciprocal(out=s8[:, 2:3], in_=s8[:, 1:2]), reads=[R_s8], writes=[R_s8])
                        P.dve(lambda e, ht=ht, zn=zn, s8=s8: e.scalar_tensor_tensor(out=zn[:], in0=ht[:], scalar=s8[:, 2:3], in1=gb[:],
                                                                                   op0=ALU.mult, op1=ALU.mult),
                              reads=[R_ht, R_s8, R_gb], writes=[R_zn])
                        for half in range(2):
                            for c4 in range(4):
                                kc = half * 4 + c4
                                P.pe(lambda e, zn=zn, kc=kc, c4=c4, half=half: e.transpose(
                                    psb[half][:, c4 * 128:(c4 + 1) * 128], zn[:, kc * 128:(kc + 1) * 128], ident[:]),
                                    reads=[R_zn, R_ident], writes=[R_ps[half]])
                        P.act(lambda e, zt_=zt_, s=s: e.copy(out=zt_[:, 0:4, s * 128:(s + 1) * 128],
                                                             in_=psb[0][:].rearrange("p (c t) -> p c t", c=4)),
                              reads=[R_ps[0]], writes=[R_z])
                        P.dve(lambda e, zt_=zt_, s=s: e.tensor_copy(out=zt_[:, 4:8, s * 128:(s + 1) * 128],
                                                                    in_=psb[1][:].rearrange("p (c t) -> p c t", c=4)),
                              reads=[R_ps[1]], writes=[R_z])

                def proj_mm(bank, c0, ncol, zt_, R_z, W):
                    for kc in range(8):
                        P.pe(lambda e, kc=kc: e.matmul(psb[bank][0:ncol, 0:W], lhsT=win[:, kc, c0:c0 + ncol], rhs=zt_[:, kc, 0:W],
                                                       start=(kc == 0), stop=(kc == 7)),
                             reads=[R_wint, R_z], writes=[R_ps[bank]])

                rot = {"n": 0, "q": 0, "v": 0, "c": 0}
                lnq = {"ops": []}

                def lnpull(n=1):
                    if lnq["ops"]:
                        P.ops.extend(lnq["ops"][:n])
                        lnq["ops"] = lnq["ops"][n:]

                def main(b):
                    t0, W = blocks[b]
                    zt_, R_z = zT[b % 2]
                    for hf in range(2):
                        proj_mm(2, hf * 128, 128, zt_, R_z, W)
                        proj_mm(3, 256 + hf * 128, 128, zt_, R_z, W)
                        P.act(lambda e: e.activation(out=sg[:, 0:W], in_=psb[3][:, 0:W], func=AF.Sigmoid),
                              reads=[R_ps[3]], writes=[R_sg])
                        P.dve(lambda e, hf=hf: e.tensor_tensor(out=uT[:, hf, 32 + t0:32 + t0 + W], in0=psb[2][:, 0:W], in1=sg[:, 0:W], op=ALU.mult),
                              reads=[R_ps[2], R_sg], writes=[R_uT])
                    for kind in range(2):
                        for c in range(4):
                            bank = 4 + rot["n"] % 2
                            rot["n"] += 1
                            lnpull()
                            proj_mm(bank, 512 + kind * 512 + c * 128, 128, zt_, R_z, W)
                            stg, R_stg = qkst[rot["q"] % 2]
                            rot["q"] += 1
                            if kind == 0:
                                P.act(lambda e, stg=stg, bank=bank: e.mul(out=stg[:, 0:W], in_=psb[bank][:, 0:W], mul=0.125),
                                      reads=[R_ps[bank]], writes=[R_stg])
                            else:
                                P.dve(lambda e, stg=stg, bank=bank: e.tensor_copy(out=stg[:, 0:W], in_=psb[bank][:, 0:W]),
                                      reads=[R_ps[bank]], writes=[R_stg])
                            dst, R_dst = (qT_d, R_q) if kind == 0 else (kT_d, R_k)
                            for hh in range(2):
                                P.dma(stq_a, dst[2 * c + hh, 0:64, t0:t0 + W], stg[hh * 64:(hh + 1) * 64, 0:W],
                                      reads=[R_stg], writes=[R_dst])
                    for s in range(W // 128):
                        bank = 4 + rot["n"] % 2
                        rot["n"] += 1
                        lnpull()
                        for kc in range(8):
                            P.pe(lambda e, kc=kc, s=s, bank=bank: e.matmul(psb[bank][:, :], lhsT=zt_[:, kc, s * 128:(s + 1) * 128],
                                                                rhs=win[:, kc, 1536:2048], start=(kc == 0), stop=(kc == 7)),
                                 reads=[R_wint, R_z], writes=[R_ps[bank]])
                        vt, R_vt = vst[rot["v"] % 2]
                        rot["v"] += 1
                        if s % 2 == 0:
                            P.act(lambda e, vt=vt, bank=bank: e.copy(out=vt[:], in_=psb[bank][:]), reads=[R_ps[bank]], writes=[R_vt])
                        else:
                            P.dve(lambda e, vt=vt, bank=bank: e.tensor_copy(out=vt[:], in_=psb[bank][:]), reads=[R_ps[bank]], writes=[R_vt])
                        tt = t0 + s * 128
                        P.dma(stq_a, v_d[:, tt:tt + 128, :].rearrange("h t d -> t h d"),
                              vt[:].rearrange("t (h d) -> t h d", h=NH), reads=[R_vt], writes=[R_v])
                    fbank = 4 + rot["n"] % 2
                    rot["n"] += 1
                    lnpull()
                    proj_mm(fbank, 2048, NH, zt_, R_z, W)
                    P.act(lambda e: e.activation(out=fe[:, 0:W], in_=psb[fbank][0:NH, 0:W], func=AF.Exp, scale=-1.0, bias=negb[:, 0:1]),
                          reads=[R_ps[fbank], R_negb], writes=[R_fe])
                    P.act(lambda e: e.activation(out=fe[:, 0:W], in_=fe[:, 0:W], func=AF.Ln, bias=1.0),
                          reads=[R_fe], writes=[R_fe])
                    init = 0.0 if t0 == 0 else crow[:, t0 - 1:t0]
                    P.dve(lambda e, init=init: e.tensor_tensor_scan(out=crow[:, t0:t0 + W], data0=ones8[:, 0:W], data1=fe[:, 0:W],
                                                                    initial=init, op0=ALU.mult, op1=ALU.subtract),
                          reads=[R_ones8, R_fe, R_crow], writes=[R_crow])
                    P.dve(lambda e: e.tensor_copy(out=cpos[:, 0, 0:W], in_=crow[:, t0:t0 + W]), reads=[R_crow], writes=[R_cpos])
                    P.dve(lambda e: e.tensor_tensor(out=cres[:, 0:W], in0=crow[:, t0:t0 + W], in1=cpos[:, 0, 0:W], op=ALU.subtract),
                          reads=[R_crow, R_cpos], writes=[R_cres])
                    P.dve(lambda e: e.tensor_copy(out=cpos[:, 1, 0:W], in_=cres[:, 0:W]), reads=[R_cres], writes=[R_cpos])
                    P.dve(lambda e: e.tensor_tensor(out=cres[:, 0:W], in0=cres[:, 0:W], in1=cpos[:, 1, 0:W], op=ALU.subtract),
                          reads=[R_cres, R_cpos], writes=[R_cres])
                    P.dve(lambda e: e.tensor_copy(out=cpos[:, 2, 0:W], in_=cres[:, 0:W]), reads=[R_cres], writes=[R_cpos])
                    P.act(lambda e: e.mul(out=cneg[:, :, 0:W], in_=cpos[:, :, 0:W], mul=-1.0), reads=[R_cpos], writes=[R_cneg])
                    P.dma(stq_a, qT_d[:, 64:67, t0:t0 + W], cpos[:, :, 0:W], reads=[R_cpos], writes=[R_q])
                    P.dma(stq_a, kT_d[:, 67:70, t0:t0 + W], cneg[:, :, 0:W], reads=[R_cneg], writes=[R_k])
                    for hf in range(2):
                        bank = 4 + rot["n"] % 2
                        rot["n"] += 1
                        proj_mm(bank, 2056 + hf * 128, 128, zt_, R_z, W)
                        su_, R_su_ = sust[hf]
                        P.act(lambda e, su_=su_, bank=bank: e.copy(out=su_[:, 0:W], in_=psb[bank][:, 0:W]),
                              reads=[R_ps[bank]], writes=[R_su_])
                        P.dma(stq_a, suT_d[hf * 128:(hf + 1) * 128, t0:t0 + W], su_[:, 0:W], reads=[R_su_], writes=[R_sud])
                    lnpull(1000)
                    for hf in range(2):
                        bank = 2 + hf
                        for j in range(CW):
                            P.pe(lambda e, hf=hf, j=j, bank=bank: e.matmul(psb[bank][:, 0:W], lhsT=diagw[:, hf, j, :],
                                                                           rhs=uT[:, hf, t0 + j + 2:t0 + j + 2 + W],
                                                                           start=(j == 0), stop=(j == CW - 1)),
                                 reads=[R_diagw, R_uT], writes=[R_ps[bank]])
                        P.act(lambda e, hf=hf, bank=bank: e.activation(out=ycv[:, hf, 0:W], in_=psb[bank][:, 0:W], func=AF.Identity,
                                                                       bias=cvp[:, hf, 0:1]),
                              reads=[R_ps[bank], R_cvp], writes=[R_ycv])
                        P.act(lambda e, hf=hf, bank=bank: e.activation(out=ysq[:, hf, 0:W], in_=psb[bank][:, 0:W], func=AF.Square,
                                                                       bias=cvp[:, hf, 0:1]),
                              reads=[R_ps[bank], R_cvp], writes=[R_ysq])
                    for hf in range(2):
                        P.pe(lambda e, hf=hf: e.matmul(psb[6][:, 0:W], lhsT=onesf[:], rhs=ycv[:, hf, 0:W], start=(hf == 0), stop=(hf == 1)),
                             reads=[R_onesf, R_ycv], writes=[R_ps[6]])
                    for hf in range(2):
                        P.pe(lambda e, hf=hf: e.matmul(psb[7][:, 0:W], lhsT=onesf[:], rhs=ysq[:, hf, 0:W], start=(hf == 0), stop=(hf == 1)),
                             reads=[R_onesf, R_ysq], writes=[R_ps[7]])
                    P.side_begin()
                    P.act(lambda e: e.copy(out=mean[:, 0:W], in_=psb[6][:, 0:W]), reads=[R_ps[6]], writes=[R_mean])
                    P.dve(lambda e: e.tensor_tensor(out=var[:, 0:W], in0=mean[:, 0:W], in1=mean[:, 0:W], op=ALU.mult),
                          reads=[R_mean], writes=[R_var])
                    P.dve(lambda e: e.tensor_tensor(out=var[:, 0:W], in0=psb[7][:, 0:W], in1=var[:, 0:W], op=ALU.subtract),
                          reads=[R_ps[7], R_var], writes=[R_var])
                    P.act(lambda e: e.activation(out=var[:, 0:W], in_=var[:, 0:W], func=AF.Sqrt, bias=EPS),
                          reads=[R_var], writes=[R_var])
                    P.dve(lambda e: e.reciprocal(out=rstd[:, 0:W], in_=var[:, 0:W]), reads=[R_var], writes=[R_rstd])
                    for hf in range(2):
                        P.dve(lambda e, hf=hf: e.tensor_tensor(out=ycv[:, hf, 0:W], in0=ycv[:, hf, 0:W], in1=mean[:, 0:W], op=ALU.subtract),
                              reads=[R_ycv, R_mean], writes=[R_ycv])
                        P.dve(lambda e, hf=hf: e.tensor_tensor(out=ycv[:, hf, 0:W], in0=ycv[:, hf, 0:W], in1=rstd[:, 0:W], op=ALU.mult),
                              reads=[R_ycv, R_rstd], writes=[R_ycv])
                        co, R_co = cvo[rot["c"] % 2]
                        rot["c"] += 1
                        P.act(lambda e, hf=hf, co=co: e.activation(out=co[:, 0:W], in_=ycv[:, hf, 0:W], func=AF.Silu,
                                                                   scale=cvp[:, hf, 1:2], bias=cvp[:, hf, 2:3]),
                              reads=[R_ycv, R_cvp], writes=[R_co])
                        P.dma(stq_a, mixT_d[hf * 128:(hf + 1) * 128, t0:t0 + W], co[:, 0:W], reads=[R_co], writes=[R_mixc])
                    lnq["ops"] = P.side_end()

                prep(0)
                for b in range(len(blocks)):
                    if b + 1 < len(blocks):
                        prep(b + 1)
                    main(b)
                P.ops.extend(lnq["ops"])
                lnq["ops"] = []
                P.flush()
            if stop_after == ("A", l):
                break

            with contextlib.ExitStack() as st:
                maskneg, R_mask = mk(st, "maskneg", (128, 128), F32)
                iota, R_iota = mk(st, "iota", (128, 128), F32)
                onesm, R_onesm = mk(st, "onesm", (128, 128), F32)
                for nm, tl, rr in (("c_maskneg", maskneg, R_mask), ("c_iota", iota, R_iota), ("c_onesm", onesm, R_onesm)):
                    P.dma("sp", tl[:], cst[nm], writes=[rr])
                Kt = [mk(st, "Kt%d" % i, (70, TP), BF16) for i in range(2)]
                Qt = [mk(st, "Qt%d" % i, (70, TP), BF16) for i in range(2)]
                Vt = [mk(st, "Vt%d" % i, (128, NT, 65), BF16) for i in range(2)]
                PT = [mk(st, "PT%d" % i, (128, 512), BF16) for i in range(3)]
                identb, R_identb = mk(st, "identb", (128, 128), BF16)
                maskb, R_maskb = mk(st, "maskb", (128, 128), BF16)
                P.dve(lambda e: e.tensor_copy(out=identb[:], in_=ident[:]), reads=[R_ident], writes=[R_identb])
                P.dve(lambda e: e.tensor_copy(out=maskb[:], in_=maskneg[:]), reads=[R_mask], writes=[R_maskb])
                ot, R_ot = mk(st, "ot", (65, 512), F32)
                rl, R_rl = mk(st, "rl", (64, 512), F32)
                yt, R_yt = mk(st, "yt", (64, 512), F32)
                ysq2, R_ysq2 = mk(st, "ysq2", (64, 512), F32)
                ybf = [mk(st, "ybf%d" % i, (64, 512), BF16) for i in range(2)]
                sel, R_sel = mk(st, "sel", (65, 64), F32)
                tl4, R_tl4 = mk(st, "tl4", (128, 4), F32)
                gatt, R_gatt = mk(st, "gatt", (64, NH), F32)
                P.dve(lambda e: e.memset(sel[:], 0.0), writes=[R_sel])
                P.dve(lambda e: e.memset(sel[64:65, :], 1.0), writes=[R_sel])
                for i in range(2):
                    P.dve(lambda e, i=i: e.memset(Vt[i][0][:, :, 64:65], 1.0), writes=[Vt[i][1]])
                P.dma("sp", gatt[:], prm["att_norm_g"][l].rearrange("(h d) -> d h", d=64), writes=[R_gatt])
                cnt = {"s": 0, "p": 0, "m": 0, "y": 0}
                inter = {"side": [], "pos": 0, "per": 0, "pair": 0, "lastdue": 0, "blk": 0}
                epi_q = []

                def epi_tick():
                    inter["pair"] += 1
                    while epi_q and epi_q[0][1] <= inter["pair"]:
                        P.ops.append(epi_q.pop(0)[0])

                def pull_side():
                    sd = inter["side"]
                    p0 = inter["pos"]
                    if p0 < inter["nset"]:
                        p1 = p0 + 1
                    else:
                        inter["acc"] += inter["rate"]
                        n = int(inter["acc"])
                        inter["acc"] -= n
                        p1 = min(len(sd), p0 + n)
                    if p1 > p0:
                        P.ops.extend(sd[p0:p1])
                        inter["pos"] = p1
                def att_block(hd, K_, R_K, Q_, R_Q, V_, R_V, t0, W):
                    while epi_q and epi_q[0][2] <= inter["blk"] - 2:
                        P.ops.append(epi_q.pop(0)[0])
                    nk = (t0 + W) // 128
                    obank = 2 + (cnt["y"] % 2)

                    def s_mm(ki):
                        k0 = ki * 128
                        qs = max(t0, k0)
                        Wq = t0 + W - qs
                        bank = cnt["s"] % 2
                        cnt["s"] += 1
                        diag = (k0 >= t0)
                        P.pe(lambda e: e.matmul(psb[bank][:, 0:Wq], lhsT=K_[:, k0:k0 + 128], rhs=Q_[:, qs:qs + Wq], start=True, stop=(not diag)),
                             reads=[R_K, R_Q], writes=[R_ps[bank]])
                        if diag:
                            P.pe(lambda e: e.matmul(psb[bank][:, 0:128], lhsT=identb[:], rhs=maskb[:], start=False, stop=True),
                                 reads=[R_identb, R_maskb], writes=[R_ps[bank]])
                        pt, R_pt = PT[cnt["p"] % 3]
                        cnt["p"] += 1
                        P.act(lambda e: e.activation(out=pt[:, 0:Wq], in_=psb[bank][:, 0:Wq], func=AF.Exp),
                              reads=[R_ps[bank]], writes=[R_pt])
                        return (ki, qs, Wq, pt, R_pt)

                    def pv_mm(info):
                        ki, qs, Wq, pt, R_pt = info
                        P.pe(lambda e: e.matmul(psb[obank][0:65, qs - t0:qs - t0 + Wq], lhsT=V_[:, ki, :], rhs=pt[:, 0:Wq],
                                                start=(ki == 0), stop=(ki == nk - 1)),
                             reads=[R_V, R_pt], writes=[R_ps[obank]])

                    pend = s_mm(0)
                    for ki in range(nk):
                        nxt = s_mm(ki + 1) if ki + 1 < nk else None
                        pv_mm(pend)
                        pend = nxt
                        pull_side()
                        epi_tick()
                    cnt["y"] += 1
                    P.side_begin()
                    ns = W // 128
                    ti0 = t0 // 128
                    P.act(lambda e: e.copy(out=ot[:, 0:W], in_=psb[obank][0:65, 0:W]), reads=[R_ps[obank]], writes=[R_ot])
                    P.act(lambda e: e.activation(out=ysq2[:, 0:W], in_=ot[0:64, 0:W], func=AF.Square), reads=[R_ot], writes=[R_ysq2])
                    P.pe(lambda e: e.matmul(psb[obank][0:64, 0:W], lhsT=sel[:], rhs=ot[:, 0:W], start=True, stop=True),
                         reads=[R_sel, R_ot], writes=[R_ps[obank]])
                    for s in range(ns):
                        P.pe(lambda e, s=s: e.matmul(psb[4][:, s:s + 1], lhsT=ysq2[:, s * 128:(s + 1) * 128], rhs=onescol[0:64, :],
                                                     start=True, stop=True),
                             reads=[R_ysq2, R_onescol], writes=[R_ps[4]])
                    for s in range(ns):
                        P.pe(lambda e, s=s: e.matmul(psb[4][:, 4 + s:5 + s], lhsT=ot[:, s * 128:(s + 1) * 128], rhs=sel[:, 0:1],
                                                     start=True, stop=True),
                             reads=[R_ot, R_sel], writes=[R_ps[4]])
                    P.dve(lambda e: e.reciprocal(out=rl[:, 0:W], in_=psb[obank][0:64, 0:W]), reads=[R_ps[obank]], writes=[R_rl])
                    yb, R_yb = ybf[cnt["y"] % 2]
                    P.dve(lambda e: e.scalar_tensor_tensor(out=yb[:, 0:W], in0=ot[0:64, 0:W], scalar=gatt[:, hd:hd + 1], in1=rl[:, 0:W],
                                                           op0=ALU.mult, op1=ALU.mult),
                          reads=[R_ot, R_gatt, R_rl], writes=[R_yb])
                    P.dma("sp", mixT_d[256 + hd * 64:256 + (hd + 1) * 64, t0:t0 + W], yb[:, 0:W], reads=[R_yb], writes=[R_mixa])
                    P.dve(lambda e: e.reciprocal(out=tl4[:, 0:ns], in_=psb[4][:, 4:4 + ns]), reads=[R_ps[4]], writes=[R_tl4])
                    P.dve(lambda e: e.tensor_tensor(out=tl4[:, 0:ns], in0=tl4[:, 0:ns], in1=tl4[:, 0:ns], op=ALU.mult), reads=[R_tl4], writes=[R_tl4])
                    P.dve(lambda e: e.tensor_tensor(out=rsatt[:, ti0:ti0 + ns, hd], in0=psb[4][:, 0:ns], in1=tl4[:, 0:ns], op=ALU.mult),
                          reads=[R_ps[4], R_tl4], writes=[R_rsatt])
                    eops = P.side_end()
                    delays = [1, 1, 1] + [1] + [0] * (ns - 1) + [0] * ns + [1, 4, 1, 0, 0, 0]
                    assert len(delays) == len(eops), (len(delays), len(eops))
                    due = max(inter["pair"], inter["lastdue"])
                    for o_, d_ in zip(eops, delays):
                        due += d_
                        epi_q.append((o_, due, inter["blk"]))
                    inter["lastdue"] = due
                    inter["blk"] += 1

                def att_loads(hd):
                    K_, R_K = Kt[hd % 2]
                    Q_, R_Q = Qt[hd % 2]
                    V_, R_V = Vt[hd % 2]
                    P.dma("sp", K_[:], kT_d[hd], reads=[R_k], writes=[R_K])
                    P.dma("sp", Q_[:], qT_d[hd], reads=[R_q], writes=[R_Q])
                    P.dma("sp", V_[:, :, 0:64], v_d[hd].rearrange("(i p) d -> p i d", p=128), reads=[R_v], writes=[R_V])

                def run_attention(side):
                    npairs = NH * sum((t0 + W) // 128 for (t0, W) in blocks)
                    inter["side"] = side
                    inter["pos"] = 0
                    inter["nset"] = s5_nsetup
                    inter["rate"] = (len(side) - s5_nsetup) / max(1.0, 0.93 * npairs - s5_nsetup)
                    inter["acc"] = 0.0
                    att_loads(0)
                    for hd in range(NH):
                        if hd + 1 < NH:
                            att_loads(hd + 1)
                        K_, R_K = Kt[hd % 2]
                        Q_, R_Q = Qt[hd % 2]
                        V_, R_V = Vt[hd % 2]
                        for (t0, W) in blocks:
                            att_block(hd, K_, R_K, Q_, R_Q, V_, R_V, t0, W)
                    for q_ in epi_q:
                        P.ops.append(q_[0])
                    del epi_q[:]
                    P.ops.extend(side[inter["pos"]:])
                    inter["pos"] = len(side)

                P.side_begin()
                sp8 = {}
                for nm in ("lr", "li", "dt", "th", "r", "ang", "cs", "sn", "are", "aim", "nr", "den", "fre", "fim", "t1", "t2",
                           "xlr", "xli", "u1", "u2"):
                    sp8[nm] = mk(st, "s8_" + nm, (128, 8), F32)
                angi, R_angi = mk(st, "angi", (128, 8), I32)
                bre, R_bre = mk(st, "bre", (128, 8, 16), F32)
                bim, R_bim = mk(st, "bim", (128, 8, 16), F32)
                bbre, R_bbre = mk(st, "bbre", (128, 8, 16), F32)
                bbim, R_bbim = mk(st, "bbim", (128, 8, 16), F32)
                tb, R_tb = mk(st, "tb", (128, 8, 16), F32)
                Xb, R_Xb = mk(st, "Xb", (128, 16, 128), F32)
                Btab, R_Btab = mk(st, "Btab", (128, 16, 128), BF16)
                Yc = [mk(st, "Yc%d" % i, (128, 128), F32) for i in range(2)]
                cT = [mk(st, "cT%d" % i, (128, 8, 16), F32) for i in range(2)]
                Ctab, R_Ctab = mk(st, "Ctab", (128, 16, 128), BF16)
                dcol, R_dcol = mk(st, "dcol", (128, 2, 8), F32)
                Dtab, R_Dtab = mk(st, "Dtab", (128, 2, 128), BF16)
                Gf, R_Gf = mk(st, "Gf", (128, 2, 128), F32)
                Gtab, R_Gtab = mk(st, "Gtab", (128, 2, 128), BF16)
                ang, R_ang = mk(st, "angf", (128, 1024), F32)
                angn, R_angn = mk(st, "angn", (128, 1024), F32)
                angq, R_angq = mk(st, "angq", (128, 1024), I32)
                cosT, R_cos = mk(st, "cosT", (128, 1024), F32)
                sinT, R_sin = mk(st, "sinT", (128, 1024), F32)
                rtab, R_rtab = mk(st, "rtab", (128, 1024), F32)
                tA, R_tA = mk(st, "tA", (128, 1024), F32)
                tB, R_tB = mk(st, "tB", (128, 1024), F32)
                bpreL = [mk(st, "bpre%d" % i, (128, 1024), F32) for i in range(2)]
                bpimL = [mk(st, "bpim%d" % i, (128, 1024), F32) for i in range(2)]
                wreL = [mk(st, "wre%d" % i, (128, 1024), F32) for i in range(2)]
                wimL = [mk(st, "wim%d" % i, (128, 1024), F32) for i in range(2)]
                tC, R_tC = mk(st, "tC", (128, 1024), F32)
                tD, R_tD = mk(st, "tD", (128, 1024), F32)
                xreL = [mk(st, "xre%d" % i, (128, 1024), BF16) for i in range(2)]
                ximL = [mk(st, "xim%d" % i, (128, 1024), BF16) for i in range(2)]
                zgL = [mk(st, "zg%d" % i, (128, 2, 128), F32) for i in range(2)]
                zgbL = [mk(st, "zgb%d" % i, (128, 2, 128), BF16) for i in range(2)]
                sgt, R_sgt = mk(st, "sgt", (128, 2, 128), F32)
                th2, R_th2 = mk(st, "th2", (128, 2, 128), F32)
                yoL = [mk(st, "yo%d" % i, (128, 2, 128), F32) for i in range(2)]
                ysq3, R_ysq3 = mk(st, "ysq3", (128, 2, 128), F32)
                ybs = [mk(st, "ybs%d" % i, (128, 2, 128), BF16) for i in range(2)]
                suT, R_su = mk(st, "suT", (128, 2, TP), BF16)
                P.dma("sp", suT[:], suT_d.rearrange("(hf p) t -> p hf t", p=128), reads=[R_sud], writes=[R_su])

                def S(nm):
                    return sp8[nm][0]

                def RS(nm):
                    return sp8[nm][1]

                def tt8(o, a, b, op):
                    P.dve(lambda e: e.tensor_tensor(out=S(o)[:], in0=S(a)[:], in1=S(b)[:], op=op), reads=[RS(a), RS(b)], writes=[RS(o)])

                P.dma("sp", S("lr")[:], prm["ssm_lam_re"][l].rearrange("g p -> (g p)").rearrange("(k q) -> q k", q=128), writes=[RS("lr")])
                P.dma("sp", S("li")[:], prm["ssm_lam_im"][l].rearrange("g p -> (g p)").rearrange("(k q) -> q k", q=128), writes=[RS("li")])
                ldt = prm["ssm_log_dt"][l:l + 1, :].rearrange("o (k g2) -> o g2 k", g2=2)
                for g2 in range(2):
                    P.dma("sp", S("dt")[g2 * 64:(g2 + 1) * 64, :], ldt[:, g2, :].partition_broadcast(64), writes=[RS("dt")])
                P.dma("sp", bre[:], prm["ssm_b_re"][l].rearrange("g p h -> (g p) h").rearrange("(k q) h -> q k h", q=128), writes=[R_bre])
                P.dma("sp", bim[:], prm["ssm_b_im"][l].rearrange("g p h -> (g p) h").rearrange("(k q) h -> q k h", q=128), writes=[R_bim])
                for ri, nm in enumerate(("ssm_c_re", "ssm_c_im")):
                    src = prm[nm][l].rearrange("(k g2) ho p -> k ho g2 p", g2=2)
                    for k in range(8):
                        P.dma("sp", Yc[ri][0][k * 16:(k + 1) * 16, :].rearrange("ho (g2 p) -> ho g2 p", g2=2), src[k],
                              writes=[Yc[ri][1]])
                for i, nm in enumerate(("ssm_d", "ssm_norm_g")):
                    P.dma("sp", dcol[:, :, i], prm[nm][l].rearrange("(hf p) -> p hf", p=128), writes=[R_dcol])
                P.dma("sp", dcol[:, :, 2], prm["ssm_glu_b"][l].rearrange("g k -> (g k)").rearrange("(hf p) -> p hf", p=128), writes=[R_dcol])
                P.dve(lambda e: e.memset(Gf[:], 0.0), writes=[R_Gf])
                for g in range(16):
                    hf, gl = g // 8, g % 8
                    P.dma("sp", Gf[gl * 16:(gl + 1) * 16, hf, gl * 16:(gl + 1) * 16], prm["ssm_glu_w"][l, g], reads=[R_Gf], writes=[R_Gf])
                P.dve(lambda e: e.tensor_copy(out=Gtab[:], in_=Gf[:]), reads=[R_Gf], writes=[R_Gtab])
                P.dve(lambda e: e.tensor_scalar(out=dcol[:, :, 3], in0=dcol[:, :, 2], scalar1=0.5, scalar2=None, op0=ALU.mult),
                      reads=[R_dcol], writes=[R_dcol])
                P.dve(lambda e: e.tensor_scalar(out=dcol[:, :, 4], in0=dcol[:, :, 1], scalar1=0.25, scalar2=None, op0=ALU.mult),
                      reads=[R_dcol], writes=[R_dcol])
                for hf in range(2):
                    P.dve(lambda e, hf=hf: e.tensor_scalar(out=Dtab[:, hf, :], in0=ident[:], scalar1=dcol[:, hf, 0:1], scalar2=None, op0=ALU.mult),
                          reads=[R_ident, R_dcol], writes=[R_Dtab])
                P.act(lambda e: e.activation(out=S("dt")[:], in_=S("dt")[:], func=AF.Exp), reads=[RS("dt")], writes=[RS("dt")])
                tt8("t1", "lr", "dt", ALU.mult)
                P.act(lambda e: e.activation(out=S("r")[:], in_=S("t1")[:], func=AF.Exp), reads=[RS("t1")], writes=[RS("r")])
                tt8("th", "li", "dt", ALU.mult)

                def sincos(src, R_src, n, it, R_it, tmp, R_tmp, cs_out, R_cs, sn_out, R_sn):
                    P.dve(lambda e: e.tensor_scalar(out=tmp, in0=src, scalar1=1.0 / (2 * PI), scalar2=None, op0=ALU.mult),
                          reads=[R_src], writes=[R_tmp])
                    P.dve(lambda e: e.tensor_copy(out=it, in_=tmp), reads=[R_tmp], writes=[R_it])
                    P.dve(lambda e: e.tensor_copy(out=tmp, in_=it), reads=[R_it], writes=[R_tmp])
                    P.dve(lambda e: e.scalar_tensor_tensor(out=tmp, in0=tmp, scalar=-2 * PI, in1=src, op0=ALU.mult, op1=ALU.add),
                          reads=[R_tmp, R_src], writes=[R_tmp])
                    P.dve(lambda e: e.tensor_scalar(out=tmp, in0=tmp, scalar1=PI, scalar2=-PI, op0=ALU.min, op1=ALU.max),
                          reads=[R_tmp], writes=[R_tmp])
                    P.act(lambda e: e.activation(out=sn_out, in_=tmp, func=AF.Sin), reads=[R_tmp], writes=[R_sn])
                    P.act(lambda e: e.activation(out=tmp, in_=tmp, func=AF.Abs), reads=[R_tmp], writes=[R_tmp])
                    P.dve(lambda e: e.tensor_scalar(out=tmp, in0=tmp, scalar1=-1.0, scalar2=PI / 2, op0=ALU.mult, op1=ALU.add),
                          reads=[R_tmp], writes=[R_tmp])
                    P.act(lambda e: e.activation(out=cs_out, in_=tmp, func=AF.Sin), reads=[R_tmp], writes=[R_cs])

                sincos(S("th")[:], RS("th"), 8, angi[:], R_angi, S("ang")[:], RS("ang"), S("cs")[:], RS("cs"), S("sn")[:], RS("sn"))
                tt8("are", "r", "cs", ALU.mult)
                tt8("aim", "r", "sn", ALU.mult)
                P.dve(lambda e: e.tensor_scalar(out=S("nr")[:], in0=S("are")[:], scalar1=-1.0, scalar2=None, op0=ALU.add),
                      reads=[RS("are")], writes=[RS("nr")])
                tt8("t1", "lr", "lr", ALU.mult)
                tt8("t2", "li", "li", ALU.mult)
                tt8("den", "t1", "t2", ALU.add)
                P.dve(lambda e: e.reciprocal(out=S("den")[:], in_=S("den")[:]), reads=[RS("den")], writes=[RS("den")])
                tt8("t1", "nr", "lr", ALU.mult)
                tt8("t2", "aim", "li", ALU.mult)
                tt8("fre", "t1", "t2", ALU.add)
                tt8("fre", "fre", "den", ALU.mult)
                tt8("t1", "aim", "lr", ALU.mult)
                tt8("t2", "nr", "li", ALU.mult)
                tt8("fim", "t1", "t2", ALU.subtract)
                tt8("fim", "fim", "den", ALU.mult)
                for k in range(8):
                    P.dve(lambda e, k=k: e.tensor_scalar(out=tb[:, k, :], in0=bim[:, k, :], scalar1=S("fim")[:, k:k + 1], scalar2=None, op0=ALU.mult),
                          reads=[R_bim, RS("fim")], writes=[R_tb])
                    P.dve(lambda e, k=k: e.scalar_tensor_tensor(out=bbre[:, k, :], in0=bre[:, k, :], scalar=S("fre")[:, k:k + 1], in1=tb[:, k, :],
                                                                op0=ALU.mult, op1=ALU.subtract),
                          reads=[R_bre, RS("fre"), R_tb], writes=[R_bbre])
                    P.dve(lambda e, k=k: e.tensor_scalar(out=tb[:, k, :], in0=bre[:, k, :], scalar1=S("fim")[:, k:k + 1], scalar2=None, op0=ALU.mult),
                          reads=[R_bre, RS("fim")], writes=[R_tb])
                    P.dve(lambda e, k=k: e.scalar_tensor_tensor(out=bbim[:, k, :], in0=bim[:, k, :], scalar=S("fre")[:, k:k + 1], in1=tb[:, k, :],
                                                                op0=ALU.mult, op1=ALU.add),
                          reads=[R_bim, RS("fre"), R_tb], writes=[R_bbim])
                P.dve(lambda e: e.memset(Xb[:], 0.0), writes=[R_Xb])
                for k in range(8):
                    for ri, (src, R_src) in enumerate(((bbre, R_bbre), (bbim, R_bbim))):
                        c0 = 32 * (k % 4)
                        P.dve(lambda e, k=k, ri=ri, src=src, c0=c0: e.tensor_copy(out=Xb[0:64, 2 * k + ri, c0:c0 + 16], in_=src[0:64, k, :]),
                              reads=[R_src, R_Xb], writes=[R_Xb])
                        P.dve(lambda e, k=k, ri=ri, src=src, c0=c0: e.tensor_copy(out=Xb[64:128, 2 * k + ri, c0 + 16:c0 + 32], in_=src[64:128, k, :]),
                              reads=[R_src, R_Xb], writes=[R_Xb])
                for j in range(16):
                    bank = 5 + j % 2
                    P.pe(lambda e, j=j, bank=bank: e.transpose(psb[bank][:, 0:128], Xb[:, j, :], ident[:]),
                         reads=[R_Xb, R_ident], writes=[R_ps[bank]])
                    P.act(lambda e, j=j, bank=bank: e.copy(out=Btab[:, j, :], in_=psb[bank][:, 0:128]), reads=[R_ps[bank]], writes=[R_Btab])
                P.dve(lambda e: e.memset(Ctab[:], 0.0), writes=[R_Ctab])
                for ri in range(2):
                    P.pe(lambda e, ri=ri: e.transpose(psb[5 + ri][:, 0:128], Yc[ri][0][:], ident[:]),
                         reads=[Yc[ri][1], R_ident], writes=[R_ps[5 + ri]])
                    sc = 1.0 if ri == 0 else -1.0
                    P.act(lambda e, ri=ri, sc=sc: e.mul(out=cT[ri][0][:].rearrange("q k h -> q (k h)"), in_=psb[5 + ri][:, 0:128], mul=sc),
                          reads=[R_ps[5 + ri]], writes=[cT[ri][1]])
                    for k in range(8):
                        c0 = 32 * (k % 4)
                        P.dve(lambda e, k=k, ri=ri, c0=c0: e.tensor_copy(out=Ctab[0:64, 2 * k + ri, c0:c0 + 16], in_=cT[ri][0][0:64, k, :]),
                              reads=[cT[ri][1], R_Ctab], writes=[R_Ctab])
                        P.dve(lambda e, k=k, ri=ri, c0=c0: e.tensor_copy(out=Ctab[64:128, 2 * k + ri, c0 + 16:c0 + 32], in_=cT[ri][0][64:128, k, :]),
                              reads=[cT[ri][1], R_Ctab], writes=[R_Ctab])
                for k in range(8):
                    P.dve(lambda e, k=k: e.tensor_scalar(out=ang[:, k * 128:(k + 1) * 128], in0=iota[:], scalar1=S("th")[:, k:k + 1], scalar2=None,
                                                         op0=ALU.mult),
                          reads=[R_iota, RS("th")], writes=[R_ang])
                    P.dve(lambda e, k=k: e.tensor_scalar(out=rtab[:, k * 128:(k + 1) * 128], in0=onesm[:], scalar1=S("r")[:, k:k + 1], scalar2=None,
                                                         op0=ALU.mult),
                          reads=[R_onesm, RS("r")], writes=[R_rtab])
                sincos(ang[:], R_ang, 1024, angq[:], R_angq, angn[:], R_angn, cosT[:], R_cos, sinT[:], R_sin)

                def v3(t):
                    return t[:].rearrange("p (k i) -> p k i", k=8)

                def stA(c):
                    t0 = c * 128
                    bpre, R_bpre = bpreL[c % 2]
                    bpim, R_bpim = bpimL[c % 2]
                    for hb in range(2):
                        sl = slice(hb * 512, (hb + 1) * 512)
                        for kk in range(4):
                            k = hb * 4 + kk
                            for ri in range(2):
                                P.pe(lambda e, k=k, ri=ri, kk=kk, hb=hb: e.matmul(psb[5 + ri][:, kk * 128:(kk + 1) * 128], lhsT=Btab[:, 2 * k + ri, :],
                                                                                  rhs=suT[:, hb, t0:t0 + 128], start=True, stop=True),
                                     reads=[R_Btab, R_su], writes=[R_ps[5 + ri]])
                        P.dve(lambda e, sl=sl: e.tensor_tensor(out=tA[:, sl], in0=psb[5][:], in1=cosT[:, sl], op=ALU.mult),
                              reads=[R_ps[5], R_cos], writes=[R_tA])
                        P.dve(lambda e, sl=sl: e.tensor_tensor(out=tB[:, sl], in0=psb[6][:], in1=sinT[:, sl], op=ALU.mult),
                              reads=[R_ps[6], R_sin], writes=[R_tB])
                        P.dve(lambda e, sl=sl: e.tensor_tensor(out=bpre[:, sl], in0=tA[:, sl], in1=tB[:, sl], op=ALU.add),
                              reads=[R_tA, R_tB], writes=[R_bpre])
                        P.dve(lambda e, sl=sl: e.tensor_tensor(out=tA[:, sl], in0=psb[6][:], in1=cosT[:, sl], op=ALU.mult),
                              reads=[R_ps[6], R_cos], writes=[R_tA])
                        P.dve(lambda e, sl=sl: e.tensor_tensor(out=tB[:, sl], in0=psb[5][:], in1=sinT[:, sl], op=ALU.mult),
                              reads=[R_ps[5], R_sin], writes=[R_tB])
                        P.dve(lambda e, sl=sl: e.tensor_tensor(out=bpim[:, sl], in0=tA[:, sl], in1=tB[:, sl], op=ALU.subtract),
                              reads=[R_tA, R_tB], writes=[R_bpim])

                def stB(c):
                    bpre, R_bpre = bpreL[c % 2]
                    bpim, R_bpim = bpimL[c % 2]
                    wre, R_wre = wreL[c % 2]
                    wim, R_wim = wimL[c % 2]
                    if c > 0:
                        tt8("u1", "are", "xlr", ALU.mult)
                        tt8("u2", "aim", "xli", ALU.mult)
                        tt8("u1", "u1", "u2", ALU.subtract)
                        P.dve(lambda e: e.tensor_tensor(out=v3(bpre)[:, :, 0], in0=v3(bpre)[:, :, 0], in1=S("u1")[:], op=ALU.add),
                              reads=[R_bpre, RS("u1")], writes=[R_bpre])
                        tt8("u1", "are", "xli", ALU.mult)
                        tt8("u2", "aim", "xlr", ALU.mult)
                        tt8("u1", "u1", "u2", ALU.add)
                        P.dve(lambda e: e.tensor_tensor(out=v3(bpim)[:, :, 0], in0=v3(bpim)[:, :, 0], in1=S("u1")[:], op=ALU.add),
                              reads=[R_bpim, RS("u1")], writes=[R_bpim])
                    P.dve(lambda e: e.tensor_tensor_scan(out=wre[:], data0=rtab[:], data1=bpre[:], initial=0.0, op0=ALU.mult, op1=ALU.add),
                          reads=[R_rtab, R_bpre], writes=[R_wre])
                    P.dve(lambda e: e.tensor_tensor_scan(out=wim[:], data0=rtab[:], data1=bpim[:], initial=0.0, op0=ALU.mult, op1=ALU.add),
                          reads=[R_rtab, R_bpim], writes=[R_wim])
                    if c + 1 < NT:
                        P.dve(lambda e: e.tensor_tensor(out=S("t1")[:], in0=v3(wre)[:, :, 127], in1=v3(cosT)[:, :, 127], op=ALU.mult),
                              reads=[R_wre, R_cos], writes=[RS("t1")])
                        P.dve(lambda e: e.tensor_tensor(out=S("t2")[:], in0=v3(wim)[:, :, 127], in1=v3(sinT)[:, :, 127], op=ALU.mult),
                              reads=[R_wim, R_sin], writes=[RS("t2")])
                        tt8("xlr", "t1", "t2", ALU.subtract)
                        P.dve(lambda e: e.tensor_tensor(out=S("t1")[:], in0=v3(wre)[:, :, 127], in1=v3(sinT)[:, :, 127], op=ALU.mult),
                              reads=[R_wre, R_sin], writes=[RS("t1")])
                        P.dve(lambda e: e.tensor_tensor(out=S("t2")[:], in0=v3(wim)[:, :, 127], in1=v3(cosT)[:, :, 127], op=ALU.mult),
                              reads=[R_wim, R_cos], writes=[RS("t2")])
                        tt8("xli", "t1", "t2", ALU.add)

                def stC(c):
                    wre, R_wre = wreL[c % 2]
                    wim, R_wim = wimL[c % 2]
                    xre, R_xre = xreL[c % 2]
                    xim, R_xim = ximL[c % 2]
                    P.pool(lambda e: e.tensor_tensor(out=tC[:], in0=wre[:], in1=cosT[:], op=ALU.mult), reads=[R_wre, R_cos], writes=[R_tC])
                    P.pool(lambda e: e.tensor_tensor(out=tD[:], in0=wim[:], in1=sinT[:], op=ALU.mult), reads=[R_wim, R_sin], writes=[R_tD])
                    P.pool(lambda e: e.tensor_tensor(out=xre[:], in0=tC[:], in1=tD[:], op=ALU.subtract), reads=[R_tC, R_tD], writes=[R_xre])
                    P.pool(lambda e: e.tensor_tensor(out=tC[:], in0=wre[:], in1=sinT[:], op=ALU.mult), reads=[R_wre, R_sin], writes=[R_tC])
                    P.pool(lambda e: e.tensor_tensor(out=tD[:], in0=wim[:], in1=cosT[:], op=ALU.mult), reads=[R_wim, R_cos], writes=[R_tD])
                    P.pool(lambda e: e.tensor_tensor(out=xim[:], in0=tC[:], in1=tD[:], op=ALU.add), reads=[R_tC, R_tD], writes=[R_xim])

                def stF1(c):
                    t0 = c * 128
                    xre, R_xre = xreL[c % 2]
                    xim, R_xim = ximL[c % 2]
                    zg, R_zg = zgL[c % 2]
                    zgb, R_zgb = zgbL[c % 2]
                    for hf in range(2):
                        yreg = psb[7][:, hf * 128:(hf + 1) * 128]
                        first = True
                        for kk in range(4):
                            k = hf * 4 + kk
                            for ri, (xx, R_xx) in enumerate(((xre, R_xre), (xim, R_xim))):
                                P.pe(lambda e, k=k, ri=ri, xx=xx, yreg=yreg, first=first: e.matmul(
                                    yreg, lhsT=Ctab[:, 2 * k + ri, :], rhs=xx[:, k * 128:(k + 1) * 128], start=first, stop=False),
                                    reads=[R_Ctab, R_xx], writes=[R_ps[7]])
                                first = False
                        P.pe(lambda e, hf=hf, yreg=yreg: e.matmul(yreg, lhsT=Dtab[:, hf, :], rhs=suT[:, hf, t0:t0 + 128], start=False, stop=True),
                             reads=[R_Dtab, R_su], writes=[R_ps[7]])
                    yall = psb[7][:, 0:256].rearrange("p (h t) -> p h t", h=2)
                    P.act(lambda e: e.activation(out=sgt[:], in_=yall, func=AF.Square), reads=[R_ps[7]], writes=[R_sgt])
                    P.dve(lambda e: e.tensor_scalar(out=sgt[:], in0=sgt[:], scalar1=0.044715, scalar2=1.0, op0=ALU.mult, op1=ALU.add),
                          reads=[R_sgt], writes=[R_sgt])
                    P.dve(lambda e: e.tensor_tensor(out=sgt[:], in0=sgt[:], in1=yall, op=ALU.mult), reads=[R_sgt, R_ps[7]], writes=[R_sgt])
                    P.act(lambda e: e.activation(out=sgt[:], in_=sgt[:], func=AF.Tanh, scale=0.7978845608028654), reads=[R_sgt], writes=[R_sgt])
                    P.dve(lambda e: e.scalar_tensor_tensor(out=zg[:], in0=sgt[:], scalar=1.0, in1=yall, op0=ALU.add, op1=ALU.mult),
                          reads=[R_sgt, R_ps[7]], writes=[R_zg])
                    P.dve(lambda e: e.tensor_scalar(out=zgb[:], in0=zg[:], scalar1=0.5, scalar2=None, op0=ALU.mult), reads=[R_zg], writes=[R_zgb])

                def stF2(c):
                    zg, R_zg = zgL[c % 2]
                    zgb, R_zgb = zgbL[c % 2]
                    yo, R_yo = yoL[c % 2]
                    for hf in range(2):
                        greg = psb[7][:, 256 + hf * 128:256 + (hf + 1) * 128]
                        P.pe(lambda e, hf=hf, greg=greg: e.matmul(greg, lhsT=Gtab[:, hf, :], rhs=zgb[:, hf, :], start=True, stop=True),
                             reads=[R_Gtab, R_zgb], writes=[R_ps[7]])
                    for hf in range(2):
                        greg = psb[7][:, 256 + hf * 128:256 + (hf + 1) * 128]
                        P.act(lambda e, hf=hf, greg=greg: e.activation(out=th2[:, hf, :], in_=greg, func=AF.Tanh, scale=0.5, bias=dcol[:, hf, 3:4]),
                              reads=[R_ps[7], R_dcol], writes=[R_th2])
                    P.dve(lambda e: e.scalar_tensor_tensor(out=yo[:], in0=th2[:], scalar=1.0, in1=zg[:], op0=ALU.add, op1=ALU.mult),
                          reads=[R_th2, R_zg], writes=[R_yo])

                def stF3(c):
                    t0 = c * 128
                    yo, R_yo = yoL[c % 2]
                    P.act(lambda e: e.activation(out=ysq3[:], in_=yo[:], func=AF.Square, scale=0.25), reads=[R_yo], writes=[R_ysq3])
                    for hf in range(2):
                        P.pe(lambda e, hf=hf: e.matmul(psb[7][:, 256:257], lhsT=ysq3[:, hf, :], rhs=onescol[:], start=(hf == 0), stop=(hf == 1)),
                             reads=[R_ysq3, R_onescol], writes=[R_ps[7]])
                    P.dve(lambda e: e.tensor_copy(out=rsssm[:, c:c + 1], in_=psb[7][:, 256:257]), reads=[R_ps[7]], writes=[R_rsssm])
                    yb, R_yb = ybs[c % 2]
                    for hf in range(2):
                        P.act(lambda e, hf=hf: e.activation(out=yb[:, hf, :], in_=yo[:, hf, :], func=AF.Copy, scale=dcol[:, hf, 4:5]),
                              reads=[R_yo, R_dcol], writes=[R_yb])
                        P.dma("sp", mixT_d[768 + hf * 128:768 + (hf + 1) * 128, t0:t0 + 128], yb[:, hf, :], reads=[R_yb], writes=[R_mixs])

                s5_nsetup = len(P.ops)
                stages = (stA, stB, stC, stF1, stF2, stF3)
                for i in range(NT + len(stages) - 1):
                    for si, stg in enumerate(stages):
                        cc_ = i - si
                        if 0 <= cc_ < NT:
                            stg(cc_)
                s5_ops = P.side_end()
                run_attention(s5_ops)
                if debug:
                    P.dma("sp", rs_d[:, 0:NT * 8], rsatt[:].rearrange("p t h -> p (t h)"), reads=[R_rsatt])
                    P.dma("sp", rs_d[:, NT * 8:NT * 9], rsssm[:], reads=[R_rsssm])
                P.flush()
            if stop_after == ("S", l):
                break

            with contextlib.ExitStack() as st:
                last = (l == L - 1)
                wout, R_woutt = mk(st, "wout", (128, 8, D), BF16)
                w2all, R_w2all = mk(st, "w2all", (128, NE, 2, D), BF16)
                mixT, R_mixT = mk(st, "mixT", (128, 8, 512), BF16)
                hld = [mk(st, "hld%d" % i, (128, D), F32) for i in range(2)]
                hn, R_hn = mk(st, "hn", (128, 4, D), F32)
                tnL = [mk(st, "tn%d" % i, (128, D), F32) for i in range(2)]
                gfb, R_gfb = mk(st, "gfb", (128, D), F32)
                tTfL = [mk(st, "tTf%d" % i, (128, 8, 128), F32) for i in range(2)]
                tTb, R_tTb = mk(st, "tTb", (128, 8, 512), BF16)
                actg, R_actg = mk(st, "actg", (128, NE, 2, 512), BF16)
                w13 = [mk(st, "w13_%d" % i, (128, 8, 512), BF16) for i in range(2)]
                wr, R_wr = mk(st, "wr", (128, 8, 20), F32)
                rbias, R_rbias = mk(st, "rbias", (128, 20), F32)
                selE, R_selE = mk(st, "selE", (16, NE, 128), BF16)
                gTh, R_gTh = mk(st, "gTh", (16, 512), BF16)
                gTl, R_gTl = mk(st, "gTl", (16, 512), BF16)
                gtfL = [mk(st, "gtf%d" % i, (16, 128), F32) for i in range(2)]
                s1 = [mk(st, "s1_%d" % i, (128, 512), F32) for i in range(2)]
                r8L = [mk(st, "r8_%d" % i, (128, 16), F32) for i in range(2)]
                r8, R_r8 = r8L[0]
                lgL = [mk(st, "lg%d" % i, (128, 20), F32) for i in range(2)]
                rtL = [mk(st, "rt%d" % i, (128, 64), F32) for i in range(2)]
                gatesL = [mk(st, "gates%d" % i, (128, 16), F32) for i in range(2)]
                fgb, R_fgb = mk(st, "fgb", (128, D), F32)

                for kc in range(8):
                    P.dma("sp", wout[:, kc, :], wout_b[l, kc * 128:(kc + 1) * 128, :], reads=[R_wout[l]], writes=[R_woutt])
                P.dma("sp", gfb[:], prm["norm_ffn_g"][l:l + 1, :].partition_broadcast(128), writes=[R_gfb])
                if last:
                    P.dma("sp", fgb[:], prm["final_norm_g"].rearrange("(o d) -> o d", o=1).partition_broadcast(128), writes=[R_fgb])
                P.dma("sp", wr[:, :, 0:4], prm["router_g_w"][l].rearrange("(kc p) n -> p kc n", p=128), writes=[R_wr])
                P.dma("sp", wr[:, :, 4:20], prm["router_e_w"][l].rearrange("(kc p) n -> p kc n", p=128), writes=[R_wr])
                P.dma("sp", rbias[:, 0:4], prm["router_g_b"][l:l + 1, :].partition_broadcast(128), writes=[R_rbias])
                P.dma("sp", rbias[:, 4:20], prm["router_e_b"][l:l + 1, :].partition_broadcast(128), writes=[R_rbias])
                P.dve(lambda e: e.memset(selE[:], 0.0), writes=[R_selE])
                for e_ in range(NE):
                    P.dve(lambda e, e_=e_: e.tensor_scalar(out=selE[:, e_, :], in0=selE[:, e_, :], scalar1=ident[0:16, e_:e_ + 1], scalar2=None,
                                                           op0=ALU.add),
                          reads=[R_selE, R_ident], writes=[R_selE])
                P.flush()

                def R8(i):
                    return r8[:, i:i + 1]

                wcnt = {"n": 0, "u": 0, "o": 0, "h": 0}
                castq2 = []
                if l + 1 < L:
                    P.side_begin()
                    cast_layer_small(l + 1)
                    cast_layer_big(l + 1)
                    castq2 = P.side_end()

                def moe_block(t0, W):
                    ns = W // 128
                    P.dma("sp", mixT[:, :, 0:W], mixT_d[:, t0:t0 + W].rearrange("(kc p) t -> p kc t", p=128), reads=[R_mixc, R_mixa, R_mixs], writes=[R_mixT])
                    def d_sub(s, ch):
                        ti = t0 // 128 + s
                        hl, R_hl = hld[ch]
                        r8, R_r8 = r8L[ch]
                        lg, R_lg = lgL[ch]
                        rt, R_rt = rtL[ch]
                        gates, R_gates = gatesL[ch]
                        tn, R_tn = tnL[ch]
                        tTf, R_tTf = tTfL[ch]
                        gtf, R_gtf = gtfL[ch]
                        bk = [4 * ch + i for i in range(4)]

                        def R8(i):
                            return r8[:, i:i + 1]

                        load_h(l, hl, R_hl, ti)
                        P.dve(lambda e: e.tensor_reduce(out=R8(0), in_=rsatt[:, ti, :], axis=AX.X, op=ALU.add), reads=[R_rsatt], writes=[R_r8])
                        P.act(lambda e: e.activation(out=R8(1), in_=R8(0), func=AF.Sqrt, scale=1.0 / 512.0, bias=EPS), reads=[R_r8], writes=[R_r8])
                        P.act(lambda e: e.activation(out=R8(2), in_=rsssm[:, ti:ti + 1], func=AF.Sqrt, scale=1.0 / 256.0, bias=EPS),
                              reads=[R_rsssm], writes=[R_r8])
                        P.dve(lambda e: e.reciprocal(out=r8[:, 3:5], in_=r8[:, 1:3]), reads=[R_r8], writes=[R_r8])
                        yield
                        for gi, kcs in enumerate(((0, 1), (2, 3, 4, 5), (6, 7))):
                            for nh in range(2):
                                bank = bk[(2 * gi + nh) % 4]
                                for j, kc in enumerate(kcs):
                                    P.pe(lambda e, kc=kc, nh=nh, bank=bank, j=j, n=len(kcs): e.matmul(
                                        psb[bank][:, :], lhsT=mixT[:, kc, s * 128:(s + 1) * 128], rhs=wout[:, kc, nh * 512:(nh + 1) * 512],
                                        start=(j == 0), stop=(j == n - 1)),
                                        reads=[R_mixT, R_woutt], writes=[R_ps[bank]])
                            yield
                            for nh in range(2):
                                bank = bk[(2 * gi + nh) % 4]
                                cs = slice(nh * 512, (nh + 1) * 512)
                                if gi == 0:
                                    P.dve(lambda e, cs=cs, bank=bank: e.tensor_tensor(out=hn[:, s, cs], in0=psb[bank][:], in1=hl[:, cs], op=ALU.add),
                                          reads=[R_ps[bank], R_hl], writes=[R_hn])
                                else:
                                    P.dve(lambda e, cs=cs, bank=bank, gi=gi: e.scalar_tensor_tensor(out=hn[:, s, cs], in0=psb[bank][:], scalar=R8(2 + gi),
                                                                                                    in1=hn[:, s, cs], op0=ALU.mult, op1=ALU.add),
                                          reads=[R_ps[bank], R_r8, R_hn], writes=[R_hn])
                            yield
                        P.act(lambda e: e.activation(out=tn[:], in_=hn[:, s, :], func=AF.Square, accum_out=R8(5)), reads=[R_hn], writes=[R_tn, R_r8])
                        P.act(lambda e: e.activation(out=R8(6), in_=R8(5), func=AF.Sqrt, scale=1.0 / D, bias=EPS), reads=[R_r8], writes=[R_r8])
                        yield
                        P.dve(lambda e: e.reciprocal(out=R8(7), in_=R8(6)), reads=[R_r8], writes=[R_r8])
                        P.dve(lambda e: e.scalar_tensor_tensor(out=tn[:], in0=hn[:, s, :], scalar=R8(7), in1=gfb[:], op0=ALU.mult, op1=ALU.mult),
                              reads=[R_hn, R_r8, R_gfb], writes=[R_tn])
                        yield
                        for half in range(2):
                            bank = bk[2 + half]
                            for c4 in range(4):
                                kc = half * 4 + c4
                                P.pe(lambda e, kc=kc, c4=c4, bank=bank: e.transpose(psb[bank][:, c4 * 128:(c4 + 1) * 128],
                                                                                   tn[:, kc * 128:(kc + 1) * 128], ident[:]),
                                     reads=[R_tn, R_ident], writes=[R_ps[bank]])
                        yield
                        P.act(lambda e: e.copy(out=tTf[:, 0:4, :], in_=psb[bk[2]][:].rearrange("p (c t) -> p c t", c=4)), reads=[R_ps[bk[2]]], writes=[R_tTf])
                        P.dve(lambda e: e.tensor_copy(out=tTf[:, 4:8, :], in_=psb[bk[3]][:].rearrange("p (c t) -> p c t", c=4)), reads=[R_ps[bk[3]]], writes=[R_tTf])
                        yield
                        for kc in range(8):
                            P.pe(lambda e, kc=kc: e.matmul(psb[bk[0]][:, 0:20], lhsT=tTf[:, kc, :], rhs=wr[:, kc, :], start=(kc == 0), stop=(kc == 7)),
                                 reads=[R_tTf, R_wr], writes=[R_ps[bk[0]]])
                        P.act(lambda e: e.copy(out=tTb[:, :, s * 128:(s + 1) * 128], in_=tTf[:]), reads=[R_tTf], writes=[R_tTb])
                        yield
                        P.dve(lambda e: e.tensor_tensor(out=lg[:], in0=psb[bk[0]][:, 0:20], in1=rbias[:], op=ALU.add), reads=[R_ps[bk[0]], R_rbias], writes=[R_lg])
                        RW = [R_rt, R_r8, R_lg]
                        P.dve(lambda e: e.tensor_reduce(out=R8(8), in_=lg[:, 0:4], axis=AX.X, op=ALU.max), reads=RW, writes=RW)
                        P.dve(lambda e: e.tensor_scalar(out=rt[:, 0:4], in0=lg[:, 0:4], scalar1=R8(8), scalar2=None, op0=ALU.is_equal), reads=RW, writes=RW)
                        P.dve(lambda e: e.tensor_scalar(out=R8(9), in0=R8(8), scalar1=-1.0, scalar2=None, op0=ALU.mult), reads=RW, writes=RW)
                        yield
                        P.act(lambda e: e.activation(out=rt[:, 24:28], in_=lg[:, 0:4], func=AF.Exp, bias=R8(9), accum_out=R8(10)), reads=RW, writes=RW)
                        yield
                        P.dve(lambda e: e.reciprocal(out=R8(11), in_=R8(10)), reads=RW, writes=RW)
                        P.dve(lambda e: e.tensor_scalar(out=rt[:, 4:8], in0=lg[:, 4:8], scalar1=rt[:, 0:1], scalar2=None, op0=ALU.mult), reads=RW, writes=RW)
                        for g in range(1, 4):
                            P.dve(lambda e, g=g: e.scalar_tensor_tensor(out=rt[:, 4:8], in0=lg[:, 4 + 4 * g:8 + 4 * g], scalar=rt[:, g:g + 1], in1=rt[:, 4:8],
                                                                        op0=ALU.mult, op1=ALU.add), reads=RW, writes=RW)
                        P.dve(lambda e: e.tensor_reduce(out=R8(12), in_=rt[:, 4:8], axis=AX.X, op=ALU.max), reads=RW, writes=RW)
                        P.dve(lambda e: e.tensor_scalar(out=rt[:, 8:12], in0=rt[:, 4:8], scalar1=R8(12), scalar2=None, op0=ALU.is_equal), reads=RW, writes=RW)
                        P.dve(lambda e: e.scalar_tensor_tensor(out=rt[:, 12:16], in0=rt[:, 8:12], scalar=-1e30, in1=rt[:, 4:8], op0=ALU.mult, op1=ALU.add),
                              reads=RW, writes=RW)
                        P.dve(lambda e: e.tensor_reduce(out=R8(13), in_=rt[:, 12:16], axis=AX.X, op=ALU.max), reads=RW, writes=RW)
                        P.dve(lambda e: e.tensor_scalar(out=rt[:, 16:20], in0=rt[:, 12:16], scalar1=R8(13), scalar2=None, op0=ALU.is_equal), reads=RW, writes=RW)
                        P.dve(lambda e: e.tensor_tensor(out=R8(14), in0=R8(13), in1=R8(12), op=ALU.subtract), reads=RW, writes=RW)
                        yield
                        P.act(lambda e: e.activation(out=R8(14), in_=R8(14), func=AF.Exp), reads=RW, writes=RW)
                        yield
                        P.dve(lambda e: e.tensor_scalar(out=R8(15), in0=R8(14), scalar1=1.0, scalar2=None, op0=ALU.add), reads=RW, writes=RW)
                        P.dve(lambda e: e.reciprocal(out=R8(15), in_=R8(15)), reads=RW, writes=RW)
                        P.dve(lambda e: e.tensor_tensor(out=R8(15), in0=R8(15), in1=R8(11), op=ALU.mult), reads=RW, writes=RW)
                        P.dve(lambda e: e.tensor_tensor(out=R8(14), in0=R8(14), in1=R8(15), op=ALU.mult), reads=RW, writes=RW)
                        P.dve(lambda e: e.tensor_scalar(out=rt[:, 20:24], in0=rt[:, 8:12], scalar1=R8(15), scalar2=None, op0=ALU.mult), reads=RW, writes=RW)
                        P.dve(lambda e: e.scalar_tensor_tensor(out=rt[:, 20:24], in0=rt[:, 16:20], scalar=R8(14), in1=rt[:, 20:24], op0=ALU.mult, op1=ALU.add),
                              reads=RW, writes=RW)
                        for g in range(4):
                            P.dve(lambda e, g=g: e.tensor_scalar(out=gates[:, 4 * g:4 * g + 4], in0=rt[:, 20:24], scalar1=rt[:, g:g + 1], scalar2=None,
                                                                 op0=ALU.mult), reads=RW, writes=[R_gates])
                        yield
                        P.pe(lambda e: e.transpose(psb[bk[1]][0:16, 0:128], gates[:], ident[:]), reads=[R_gates, R_ident], writes=[R_ps[bk[1]]])
                        yield
                        P.act(lambda e: e.copy(out=gTh[:, s * 128:(s + 1) * 128], in_=psb[bk[1]][0:16, 0:128]), reads=[R_ps[bk[1]]], writes=[R_gTh])
                        P.dve(lambda e: e.tensor_tensor(out=gtf[:], in0=psb[bk[1]][0:16, 0:128], in1=gTh[:, s * 128:(s + 1) * 128], op=ALU.subtract),
                              reads=[R_ps[bk[1]], R_gTh], writes=[R_gtf])
                        P.dve(lambda e: e.tensor_copy(out=gTl[:, s * 128:(s + 1) * 128], in_=gtf[:]), reads=[R_gtf], writes=[R_gTl])

                    for s0 in range(0, ns, 2):
                        gens = [d_sub(s0, 0)] + ([d_sub(s0 + 1, 1)] if s0 + 1 < ns else [])
                        while gens:
                            for g_ in list(gens):
                                try:
                                    next(g_)
                                except StopIteration:
                                    gens.remove(g_)
                    if t0 == 0:
                        for e_ in range(NE):
                            P.dma("sp", w2all[:, e_, :, :], w2_b[l, e_].rearrange("(fh p) n -> p fh n", p=128), reads=[R_w2[l]], writes=[R_w2all])
                    if castq2:
                        P.ops.extend(castq2[:9])
                        del castq2[:9]
                    for e_ in range(NE):
                        wt, R_wt = w13[wcnt["n"] % 2]
                        wcnt["n"] += 1
                        P.dma("sp", wt[:], w13_b[l, e_].rearrange("(kc p) f -> p kc f", p=128), reads=[R_w13[l]], writes=[R_wt])
                        gbank = 4 + e_ % 2
                        P.pe(lambda e, e_=e_, gbank=gbank: e.matmul(psb[gbank][:, 0:W], lhsT=selE[:, e_, :], rhs=gTh[:, 0:W], start=True, stop=False),
                             reads=[R_selE, R_gTh], writes=[R_ps[gbank]])
                        P.pe(lambda e, e_=e_, gbank=gbank: e.matmul(psb[gbank][:, 0:W], lhsT=selE[:, e_, :], rhs=gTl[:, 0:W], start=False, stop=True),
                             reads=[R_selE, R_gTl], writes=[R_ps[gbank]])
                        for fh in range(2):
                            u = wcnt["u"]
                            wcnt["u"] += 1
                            b1, b3 = 2 * (u % 2), 2 * (u % 2) + 1
                            for kc in range(8):
                                P.pe(lambda e, kc=kc, fh=fh, wt=wt, b1=b1: e.matmul(psb[b1][:, 0:W], lhsT=wt[:, kc, fh * 128:(fh + 1) * 128],
                                                                                   rhs=tTb[:, kc, 0:W], start=(kc == 0), stop=(kc == 7)),
                                     reads=[R_wt, R_tTb], writes=[R_ps[b1]])
                            for kc in range(8):
                                P.pe(lambda e, kc=kc, fh=fh, wt=wt, b3=b3: e.matmul(psb[b3][:, 0:W], lhsT=wt[:, kc, 256 + fh * 128:256 + (fh + 1) * 128],
                                                                                   rhs=tTb[:, kc, 0:W], start=(kc == 0), stop=(kc == 7)),
                                     reads=[R_wt, R_tTb], writes=[R_ps[b3]])
                            s1t, R_s1 = s1[u % 2]
                            P.act(lambda e, s1t=s1t, b1=b1: e.activation(out=s1t[:, 0:W], in_=psb[b1][:, 0:W], func=AF.Silu),
                                  reads=[R_ps[b1]], writes=[R_s1])
                            P.dve(lambda e, s1t=s1t, b3=b3: e.tensor_tensor(out=s1t[:, 0:W], in0=s1t[:, 0:W], in1=psb[b3][:, 0:W], op=ALU.mult),
                                  reads=[R_s1, R_ps[b3]], writes=[R_s1])
                            P.dve(lambda e, s1t=s1t, e_=e_, fh=fh, gbank=gbank: e.tensor_tensor(out=actg[:, e_, fh, 0:W], in0=s1t[:, 0:W],
                                                                                               in1=psb[gbank][:, 0:W], op=ALU.mult),
                                  reads=[R_s1, R_ps[gbank]], writes=[R_actg])
                    for s in range(ns):
                        ti = t0 // 128 + s
                        hot, R_hot = hn[:, s, :], R_hn
                        for nh in range(2):
                            bank = 6 + nh
                            n = 0
                            for e_ in range(NE):
                                for fh in range(2):
                                    P.pe(lambda e, e_=e_, fh=fh, nh=nh, bank=bank, n=n, s=s: e.matmul(
                                        psb[bank][:, :], lhsT=actg[:, e_, fh, s * 128:(s + 1) * 128], rhs=w2all[:, e_, fh, nh * 512:(nh + 1) * 512],
                                        start=(n == 0), stop=(n == 2 * NE - 1)),
                                        reads=[R_actg, R_w2all], writes=[R_ps[bank]])
                                    n += 1
                            cs = slice(nh * 512, (nh + 1) * 512)
                            P.dve(lambda e, nh=nh, cs=cs, bank=bank, s=s: e.tensor_tensor(out=hn[:, s, cs], in0=psb[bank][:], in1=hn[:, s, cs], op=ALU.add),
                                  reads=[R_ps[bank], R_hn], writes=[R_hn])
                        if not last:
                            P.dma("pool", h_d[ti * 128:(ti + 1) * 128, :], hot, reads=[R_hot], writes=[R_h[ti]])
                        else:
                            if debug:
                                P.dma("pool", h_d[ti * 128:(ti + 1) * 128, :], hot, reads=[R_hot], writes=[R_h[ti]])
                            P.act(lambda e, hot=hot: e.activation(out=tnL[0][0][:], in_=hot, func=AF.Square, accum_out=R8(5)),
                                  reads=[R_hot], writes=[tnL[0][1], R_r8])
                            P.act(lambda e: e.activation(out=R8(6), in_=R8(5), func=AF.Sqrt, scale=1.0 / D, bias=EPS), reads=[R_r8], writes=[R_r8])
                            P.dve(lambda e: e.reciprocal(out=R8(7), in_=R8(6)), reads=[R_r8], writes=[R_r8])
                            P.dve(lambda e, hot=hot: e.scalar_tensor_tensor(out=hot, in0=hot, scalar=R8(7), in1=fgb[:], op0=ALU.mult, op1=ALU.mult),
                                  reads=[R_hot, R_r8, R_fgb], writes=[R_hot])
                            lo = max(ti * 128, 16)
                            hi = min((ti + 1) * 128, T)
                            if hi > lo:
                                P.dma("pool", out[lo - 16:hi - 16, :], hn[lo - ti * 128:hi - ti * 128, s, :], reads=[R_hot])
                for (t0, W) in blocks:
                    moe_block(t0, W)
                P.ops.extend(castq2)
                del castq2[:]
                P.flush()
        if stop_after is not None:
            P.flush()
        stats = dict(nops=P.nop_total, nwait=P.nwait, cnt=dict(P.cnt), dcnt=dict(P.dcnt))
    return nc, stats


_CACHE = {}


def kernel(**inputs):
    x = np.ascontiguousarray(inputs["x"], dtype=np.float32)
    B, SEQ, _ = x.shape
    key = (SEQ,)
    if key not in _CACHE:
        _CACHE[key] = build(SEQ)
    nc, _ = _CACHE[key]
    consts = host_consts()
    in_maps = []
    for b in range(B):
        m = {"x": x[b]}
        for k in PARAM_SHAPES:
            m[k] = np.ascontiguousarray(inputs[k], dtype=np.float32)
        m.update(consts)
        in_maps.append(m)
    res = run_bass_kernel_spmd(nc, in_maps, core_ids=list(range(B)))
    return np.stack([np.asarray(r["out"], dtype=np.float32) for r in res.results], axis=0)
```
